# Optimizing a Trainium2 kernel written in Bass

```python
import jax
import jax.numpy as jnp
from jax import lax
import numpy as np

D_MODEL = 2048
BATCH = 4
SEQ = 4096
DEPTH = 1

CTX_LEN = 256
GRID_W = 64
MIX_WIDTH = D_MODEL
MLSTM_WIDTH = MIX_WIDTH // 2
MLSTM_HEADS = 4
MLSTM_HEAD_DIM = MLSTM_WIDTH // MLSTM_HEADS
MLSTM_CHUNK = 128
CONV_WIDTH = MIX_WIDTH - MLSTM_WIDTH
CONV_KERNEL = 31
N_EXPERTS = 16
EC_CAPACITY_FACTOR = 2
EXPERT_FF = 5504
N_ADA = 6
IN_COLS = 4 * MLSTM_WIDTH + 4 * MLSTM_HEADS + 2 * CONV_WIDTH
EPS = 1e-6
F_BIAS_LO = 3.0
F_BIAS_HI = 6.0

kernel_name = 'hybrid_mlstm_conformer_ecmoe_dit_block'


def _rmsnorm(x, w):
    xf = x.astype(jnp.float32)
    xf = xf * lax.rsqrt(jnp.mean(xf * xf, axis=-1, keepdims=True) + EPS)
    return xf.astype(x.dtype) * w


def _layernorm_f32(x):
    xf = x.astype(jnp.float32)
    mu = jnp.mean(xf, axis=-1, keepdims=True)
    var = jnp.mean(jnp.square(xf - mu), axis=-1, keepdims=True)
    return (xf - mu) * lax.rsqrt(var + EPS)


def _ada_params(cvec, w_ada, b_ada):
    return jnp.split(jax.nn.silu(cvec) @ w_ada + b_ada, N_ADA, axis=-1)


def _split_projection(p):
    W, H = MLSTM_WIDTH, MLSTM_HEADS
    return jnp.split(p, [W, 2 * W, 3 * W, 4 * W, 4 * W + 4 * H], axis=-1)


def _mlstm_inputs(q, k, v, gates, b_i, b_f):
    B, S, _ = q.shape

    def heads(t):
        return t.astype(jnp.float32).reshape(B, S, MLSTM_HEADS, MLSTM_HEAD_DIM).transpose(0, 2, 1, 3)

    g = gates.astype(jnp.float32).reshape(B, S, 2, 2, MLSTM_HEADS) + jnp.stack([b_i, b_f], axis=1).astype(jnp.float32)
    g = g.transpose(2, 3, 0, 4, 1)
    ig = g[:, 0]
    lf = jax.nn.log_sigmoid(g[:, 1])
    return heads(q), heads(k) * (MLSTM_HEAD_DIM ** -0.5), heads(v), ig, lf


def _mlstm_chunk_step(state, inp):
    C, n, m = state
    q, k, v, ig, lf = inp
    L = q.shape[2]
    past_or_self = jnp.tril(jnp.ones((L, L), dtype=bool))
    b = jnp.cumsum(lf, axis=-1)
    log_d = jnp.where(past_or_self, b[..., :, None] - b[..., None, :] + ig[..., None, :], -jnp.inf)
    log_inter = b + m[..., None]
    m_row = jnp.maximum(log_inter, jnp.max(log_d, axis=-1))
    w_intra = jnp.exp(log_d - m_row[..., None])
    w_inter = jnp.exp(log_inter - m_row)
    s = jnp.einsum('bhld,bhsd->bhls', q, k) * w_intra
    num = jnp.einsum('bhls,bhse->bhle', s, v) + w_inter[..., None] * jnp.einsum('bhld,bhde->bhle', q, C)
    den = jnp.sum(s, axis=-1) + w_inter * jnp.einsum('bhld,bhd->bhl', q, n)
    den = jnp.maximum(jnp.abs(den), jnp.exp(-m_row))
    h = num / den[..., None]
    g = b[..., -1]
    log_a = g[..., None] - b + ig
    m_new = jnp.maximum(g + m, jnp.max(log_a, axis=-1))
    w_a = jnp.exp(log_a - m_new[..., None])
    decay = jnp.exp(g + m - m_new)
    C_new = decay[..., None, None] * C + jnp.einsum('bhs,bhsd,bhse->bhde', w_a, k, v)
    n_new = decay[..., None] * n + jnp.einsum('bhs,bhsd->bhd', w_a, k)
    return (C_new, n_new, m_new), h


def _mlstm_chunked(q, k, v, ig, lf, state):
    B, H, S, Dh = q.shape
    nc = S // MLSTM_CHUNK

    def chunks(t):
        return jnp.moveaxis(t.reshape(B, H, nc, MLSTM_CHUNK, *t.shape[3:]), 2, 0)

    state, h = lax.scan(_mlstm_chunk_step, state, (chunks(q), chunks(k), chunks(v), chunks(ig), chunks(lf)))
    return jnp.moveaxis(h, 0, 2).reshape(B, H, S, Dh), state


def _mlstm_bidirectional(ctx_in, lat_in):
    qc, kc, vc, igc, lfc = ctx_in
    qx, kx, vx, igx, lfx = lat_in
    B = qx.shape[0]
    zero = (jnp.zeros((B, MLSTM_HEADS, MLSTM_HEAD_DIM, MLSTM_HEAD_DIM), jnp.float32),
            jnp.zeros((B, MLSTM_HEADS, MLSTM_HEAD_DIM), jnp.float32),
            jnp.zeros((B, MLSTM_HEADS), jnp.float32))

    def flip(t):
        return jnp.flip(t, axis=2)

    hc_f, st_f = _mlstm_chunked(qc, kc, vc, igc[0], lfc[0], zero)
    hx_f, _ = _mlstm_chunked(qx, kx, vx, igx[0], lfx[0], st_f)
    hc_b, st_b = _mlstm_chunked(flip(qc), flip(kc), flip(vc), flip(igc[1]), flip(lfc[1]), zero)
    hx_b, _ = _mlstm_chunked(flip(qx), flip(kx), flip(vx), flip(igx[1]), flip(lfx[1]), st_b)
    return hc_f + flip(hc_b), hx_f + flip(hx_b)


def _mlstm_readout(h, o, norm_w):
    B, H, S, Dh = h.shape
    hn = _layernorm_f32(h).transpose(0, 2, 1, 3).reshape(B, S, H * Dh)
    return (hn * jax.nn.sigmoid(o.astype(jnp.float32))).astype(o.dtype) * norm_w


def _conformer_conv(glu, conv_w, conv_b, ln_w, ln_b):
    a, g = jnp.split(glu, 2, axis=-1)
    u = a * jax.nn.sigmoid(g)
    y = lax.conv_general_dilated(u, conv_w[:, None, :], window_strides=(1,),
                                 padding=[(CONV_KERNEL // 2, CONV_KERNEL // 2)],
                                 dimension_numbers=('NWC', 'WIO', 'NWC'),
                                 feature_group_count=CONV_WIDTH) + conv_b
    y = _layernorm_f32(y).astype(glu.dtype) * ln_w + ln_b
    return jax.nn.silu(y)


def _expert_choice_ffn(xn, w_router, w_gate, w_up, w_down):
    B, n, _ = xn.shape
    cap = EC_CAPACITY_FACTOR * n // N_EXPERTS
    aff = jax.nn.softmax((xn @ w_router).astype(jnp.float32), axis=-1)
    gate, idx = lax.top_k(jnp.swapaxes(aff, 1, 2), cap)
    bidx = jnp.arange(B)[:, None, None]
    xg = jnp.moveaxis(xn[bidx, idx], 1, 0)

    def expert(args):
        xe, wg, wu, wd = args
        return (jax.nn.silu(xe @ wg) * (xe @ wu)) @ wd

    ye = jnp.moveaxis(lax.map(expert, (xg, w_gate, w_up, w_down)), 0, 1)
    ye = ye * gate[..., None].astype(ye.dtype)
    return jnp.zeros_like(xn).at[bidx, idx].add(ye)


def setup_inputs(seed: int = 0) -> dict:
    key = jax.random.key(seed)
    ks = jax.random.split(key, 24)
    f32 = jnp.float32

    def nrm(k, shape, scale):
        return jax.random.normal(k, shape, f32) * scale

    def gain(k, shape):
        return 1.0 + 0.01 * jax.random.normal(k, shape, f32)

    D, L, H, E, F = D_MODEL, DEPTH, MLSTM_HEADS, N_EXPERTS, EXPERT_FF
    f_bias = jnp.linspace(F_BIAS_LO, F_BIAS_HI, H, dtype=f32)
    return {
        'x': nrm(ks[0], (BATCH, SEQ, D), 1.0),
        'c': nrm(ks[1], (BATCH, D), 1.0),
        'ctx': nrm(ks[2], (BATCH, CTX_LEN, D), 1.0),
        'c_ctx': nrm(ks[3], (D,), 1.0),
        'w_ada': nrm(ks[4], (L, D, N_ADA * D), D ** -0.5),
        'b_ada': nrm(ks[5], (L, N_ADA * D), 0.01),
        'norm1_w': gain(ks[6], (L, D)),
        'w_in': nrm(ks[7], (L, D, IN_COLS), D ** -0.5),
        'mlstm_b_i': nrm(ks[8], (L, 2, H), 0.1),
        'mlstm_b_f': f_bias + nrm(ks[9], (L, 2, H), 0.1),
        'mlstm_norm_w': gain(ks[10], (L, MLSTM_WIDTH)),
        'conv_w': nrm(ks[11], (L, CONV_KERNEL, CONV_WIDTH), CONV_KERNEL ** -0.5),
        'conv_b': nrm(ks[12], (L, CONV_WIDTH), 0.01),
        'conv_norm_w': gain(ks[13], (L, CONV_WIDTH)),
        'conv_norm_b': nrm(ks[14], (L, CONV_WIDTH), 0.01),
        'w_out': nrm(ks[15], (L, MIX_WIDTH, D), MIX_WIDTH ** -0.5),
        'norm2_w': gain(ks[16], (L, D)),
        'w_router': nrm(ks[17], (L, D, E), D ** -0.5),
        'w_gate': nrm(ks[18], (L, E, D, F), D ** -0.5),
        'w_up': nrm(ks[19], (L, E, D, F), D ** -0.5),
        'w_down': nrm(ks[20], (L, E, F, D), F ** -0.5),
        'final_norm_w': gain(ks[21], (D,)),
    }


def reference(x, c, ctx, c_ctx, w_ada, b_ada, norm1_w, w_in, mlstm_b_i, mlstm_b_f, mlstm_norm_w,
              conv_w, conv_b, conv_norm_w, conv_norm_b, w_out, norm2_w, w_router, w_gate, w_up,
              w_down, final_norm_w):
    B, S, _ = x.shape
    rows = S // GRID_W
    for l in range(DEPTH):
        last = l == DEPTH - 1
        sh1, sc1, g1, sh2, sc2, g2 = [t[:, None, :] for t in _ada_params(c, w_ada[l], b_ada[l])]
        csh1, csc1, cg1, csh2, csc2, cg2 = _ada_params(c_ctx, w_ada[l], b_ada[l])

        xn = _rmsnorm(x, norm1_w[l]) * (1 + sc1) + sh1
        cn = _rmsnorm(ctx, norm1_w[l]) * (1 + csc1) + csh1
        qx, kx, vx, ox, gx, glux = _split_projection(xn @ w_in[l])
        qc, kc, vc, oc, gc, gluc = _split_projection(cn @ w_in[l])
        h_ctx, h_lat = _mlstm_bidirectional(
            _mlstm_inputs(qc, kc, vc, gc, mlstm_b_i[l], mlstm_b_f[l]),
            _mlstm_inputs(qx, kx, vx, gx, mlstm_b_i[l], mlstm_b_f[l]))
        m_lat = _mlstm_readout(h_lat, ox, mlstm_norm_w[l])
        conv_lat = _conformer_conv(glux.reshape(B * rows, GRID_W, 2 * CONV_WIDTH), conv_w[l], conv_b[l],
                                   conv_norm_w[l], conv_norm_b[l]).reshape(B, S, CONV_WIDTH)
        x = x + g1 * (jnp.concatenate([m_lat, conv_lat], axis=-1) @ w_out[l])

        xn2 = _rmsnorm(x, norm2_w[l]) * (1 + sc2) + sh2
        x = x + g2 * _expert_choice_ffn(xn2, w_router[l], w_gate[l], w_up[l], w_down[l])

        if not last:
            m_ctx = _mlstm_readout(h_ctx, oc, mlstm_norm_w[l])
            conv_ctx = _conformer_conv(gluc, conv_w[l], conv_b[l], conv_norm_w[l], conv_norm_b[l])
            ctx = ctx + cg1 * (jnp.concatenate([m_ctx, conv_ctx], axis=-1) @ w_out[l])
            cn2 = _rmsnorm(ctx, norm2_w[l]) * (1 + csc2) + csh2
            ctx = ctx + cg2 * _expert_choice_ffn(cn2, w_router[l], w_gate[l], w_up[l], w_down[l])
    return _rmsnorm(x, final_norm_w)
```

```python
import numpy as np
from contextlib import ExitStack
import concourse.bass as bass
import concourse.mybir as mybir
from concourse.bass_utils import run_bass_kernel_spmd

F32 = mybir.dt.float32
BF16 = mybir.dt.bfloat16
I32 = mybir.dt.int32
AF = mybir.ActivationFunctionType
ALU = mybir.AluOpType
AX = mybir.AxisListType

D = 2048
DC = 16
NH = 4
DH = 256
W = 1024
CW = 1024
KT = 31
EL = 8
CAP = 512
NOWN = 2048
NCTX = 256
NCH = 34
EPS = 1e-6
INC = 6160
NBIS = 28


class Res:
    __slots__ = ("w", "r")

    def __init__(self):
        self.w = None
        self.r = {}


class Tile:
    def __init__(self, t, name="", onchip=False):
        self.t = t
        self.res = Res()
        self.name = name
        self.onchip = onchip


class Eng:
    def __init__(self, name, e, sem):
        self.name = name
        self.e = e
        self.sem = sem
        self.n = 0
        self.waited = {}


class DS:
    def __init__(self, sem):
        self.sem = sem
        self.n = 0


class KB:
    def __init__(self, nc):
        self.nc = nc
        self.E = {}
        for name, e in (("pe", nc.tensor), ("act", nc.scalar), ("dve", nc.vector),
                        ("pool", nc.gpsimd), ("sp", nc.sync)):
            self.E[name] = Eng(name, e, nc.alloc_semaphore(name="sem_" + name))
        self.ds = {}
        self.free_ds = []
        self.keep = {"misc", "yz"}
        self.nosync = set()
        self.tiles = []
        self.ccsem = nc.alloc_semaphore(name="sem_cc")
        self.ccn = 0
        self._bregs = {}

    def tile(self, es, name, shape, dt, psum=False):
        if psum:
            t = es.enter_context(self.nc.psum_tensor(name, list(shape), dt))
        else:
            t = es.enter_context(self.nc.sbuf_tensor(name, list(shape), dt))
        T = Tile(t, name, True)
        self.tiles.append(T)
        return T

    def dram(self, name, shape, dt, **kw):
        T = Tile(self.nc.dram_tensor(name, list(shape), dt, **kw), name)
        self.tiles.append(T)
        return T

    def _waits(self, en, r, w):
        E = self.E[en]
        need = {}

        def add(ev):
            if ev is None:
                return
            sem, val, owner = ev
            if owner == "pe" and en == "pe":
                return
            k = id(sem)
            if k not in need or need[k][1] < val:
                need[k] = (sem, val)
        for x in r:
            add(x.res.w)
        for x in w:
            add(x.res.w)
            for ev in x.res.r.values():
                add(ev)
        for sem, val in need.values():
            if E.waited.get(id(sem), 0) < val:
                E.e.wait_ge(sem, val)
                E.waited[id(sem)] = val

    def _post(self, ev, r, w):
        for x in r:
            x.res.r[id(ev[0])] = ev
        for x in w:
            x.res.w = ev
            x.res.r = {}

    def op(self, en, fn, r=(), w=()):
        E = self.E[en]
        self._waits(en, r, w)
        ins = fn(E.e)
        E.n += 1
        ins.then_inc(E.sem, 1)
        self._post((E.sem, E.n, en), r, w)

    def dsem(self, key):
        if key not in self.ds:
            if self.free_ds:
                self.ds[key] = self.free_ds.pop()
            else:
                self.ds[key] = DS(self.nc.alloc_semaphore(name="dsem%d" % len(self.ds)))
        return self.ds[key]

    def breg(self, val):
        if val not in self._bregs:
            self._bregs[val] = self.nc.gpsimd.to_reg(val)
        return self._bregs[val]

    def _key(self, sem, r, w):
        if sem is not None:
            return sem
        for x in w:
            if x.onchip:
                return x.name
        for x in r:
            if x.onchip:
                return x.name
        return "misc"

    def end_phase(self):
        self.sync_all()
        for key in list(self.ds.keys()):
            if key not in self.keep:
                self.free_ds.append(self.ds.pop(key))

    def dma(self, en, out, in_, r=(), w=(), sem=None, **kw):
        E = self.E[en]
        self._waits(en, r, w)
        ins = E.e.dma_start(out=out, in_=in_, **kw)
        d = self.dsem(self._key(sem, r, w))
        d.n += 16
        ins.then_inc(d.sem, 16)
        self._post((d.sem, d.n, None), r, w)

    def idma(self, out, in_, out_off=None, in_off=None, bounds=0, r=(), w=(), sem=None, cop=None):
        E = self.E["pool"]
        self._waits("pool", r, w)
        kw = {}
        if cop is not None:
            kw["compute_op"] = cop
        ins = E.e.indirect_dma_start(
            out=out,
            out_offset=None if out_off is None else bass.IndirectOffsetOnAxis(ap=out_off, axis=0),
            in_=in_,
            in_offset=None if in_off is None else bass.IndirectOffsetOnAxis(ap=in_off, axis=0),
            bounds_check=self.breg(bounds), oob_is_err=False, **kw)
        d = self.dsem(self._key(sem, r, w))
        d.n += 16
        ins.then_inc(d.sem, 16)
        self._post((d.sem, d.n, None), r, w)

    def sync_all(self):
        for E in self.E.values():
            for Fn in self.E.values():
                if Fn is not E and Fn.n > 0 and E.waited.get(id(Fn.sem), 0) < Fn.n:
                    E.e.wait_ge(Fn.sem, Fn.n)
                    E.waited[id(Fn.sem)] = Fn.n
            for key, d in self.ds.items():
                if key in self.nosync:
                    continue
                if d.n > 0 and E.waited.get(id(d.sem), 0) < d.n:
                    E.e.wait_ge(d.sem, d.n)
                    E.waited[id(d.sem)] = d.n
        for T in self.tiles:
            if getattr(T, "persist", False):
                continue
            T.res.w = None
            T.res.r = {}

    def pair_barrier(self, src, dst):
        self.sync_all()
        self.E["pool"].e.collective_compute(
            "AllReduce", ALU.add, replica_groups=[[0, 1], [2, 3], [4, 5], [6, 7]],
            ins=[src.t.ap().opt()], outs=[dst.t.ap().opt()]).then_inc(self.ccsem)
        self.ccn += 1
        for E in self.E.values():
            E.e.wait_ge(self.ccsem, self.ccn)


def bcast_rows(h, off, n, parts=128):
    a = h.ap()
    return bass.AP(tensor=a.tensor, offset=off, ap=[[0, parts], [1, n]])


def build(FF=5504, dbg=False):
    nc = bass.Bass("TRN2", target_bir_lowering=False)
    k = KB(nc)
    NFC = FF // 128

    def din(name, shape, dt=F32):
        return Tile(nc.dram_tensor(name, list(shape), dt, kind="ExternalInput"), name)

    xall = din("xall", [4096, D])
    ctxl = din("ctxl", [NCTX, D])
    cs = din("cs", [128, DC, 2])
    w_ada = din("w_ada", [D, 6 * D])
    b_ada = din("b_ada", [1, 6 * D])
    n1w = din("n1w", [1, D])
    n2w = din("n2w", [1, D])
    fnw = din("fnw", [1, D])
    w_in = din("w_in", [D, INC])
    gbias = din("gbias", [1, 16])
    mnw = din("mnw", [1, W])
    convw = din("convw", [128, 8, KT])
    convb = din("convb", [128, 8])
    cnw = din("cnw", [128, 8])
    cnb = din("cnb", [128, 8])
    w_out = din("w_out", [D, D])
    w_r = din("w_r", [128, DC, 16])
    wg = din("wg", [EL, D, FF])
    wu = din("wu", [EL, D, FF])
    wd = din("wd", [EL, FF, D])
    consts = din("consts", [128, 5 * 128])
    own_gid = din("own_gid", [128, 16], I32)
    own_gid2 = din("own_gid2", [128, 16], I32)
    zrows = din("zrows", [128, 32], I32)
    yrowsA = din("yrowsA", [128, 16], I32)
    yrowsB = din("yrowsB", [128, 16], I32)
    affrows = din("affrows", [128, 32], I32)
    gid_f = din("gid_f", [128, 32])
    ybase = din("ybase", [128, 1])
    out = Tile(nc.dram_tensor("out", [NOWN, D], F32, kind="ExternalOutput"), "out")
    k.tiles += [xall, ctxl, out]

    ada_d = k.dram("ada_d", [2, 6 * D], F32)
    qT_d = k.dram("qT_d", [16, 128, 1024], BF16)
    kT_d = k.dram("kT_d", [NCH, 128, 1024], BF16)
    k_d = k.dram("k_d", [NCH * 128, W], BF16)
    v_d = k.dram("v_d", [NCH * 128, W], BF16)
    o_d = k.dram("o_d", [NOWN, W], F32)
    g_d = k.dram("g_d", [NCH * 128, 16], F32)
    uT_d = k.dram("uT_d", [8, 128, NOWN], BF16)
    h1_d = k.dram("h1_d", [NOWN, W], F32)
    mixT_d = k.dram("mixT_d", [16, 128, NOWN], BF16)
    x1_d = k.dram("x1_d", [NOWN, D], F32)
    xn2_sh = k.dram("xn2_sh", [4096, D], BF16, addr_space="Shared")
    aff_sh = k.dram("aff_sh", [8192, 16], F32, addr_space="Shared")
    yacc_sh = k.dram("yacc_sh", [8192, D], F32, addr_space="Shared")
    tokg_d = [k.dram(f"tokg_d{j}", [CAP, 2], F32) for j in range(EL)]
    NPRE = 0
    RUN = 1376 if FF % 1376 == 0 else FF
    wgb = wub = wdb = None
    if NPRE:
        wgb = k.dram("wgb", [NPRE, D, FF], BF16)
        wub = k.dram("wub", [NPRE, D, FF], BF16)
        wdb = k.dram("wdb", [NPRE, FF, D], BF16)
    bsrc = k.dram("bsrc", [128, 128], F32)
    bdst = k.dram("bdst", [128, 128], F32)
    if dbg:
        dbg_x1 = Tile(nc.dram_tensor("dbg_x1", [NOWN, D], F32, kind="ExternalOutput"))
        dbg_mix = Tile(nc.dram_tensor("dbg_mix", [16, 128, NOWN], BF16, kind="ExternalOutput"))
        dbg_aff = Tile(nc.dram_tensor("dbg_aff", [8192, 16], F32, kind="ExternalOutput"))

    with ExitStack() as g:
        cst = k.tile(g, "cst", [128, 5 * 128], F32)
        ident = cst.t[:, 0:128]
        triF = cst.t[:, 128:256]
        triB = cst.t[:, 256:384]
        ones = cst.t[:, 384:512]
        strictL = cst.t[:, 512:640]
        cbf = k.tile(g, "cbf", [128, 3 * 128], BF16)
        identb = cbf.t[:, 0:128]
        maskFb = cbf.t[:, 128:256]
        maskBb = cbf.t[:, 256:384]
        idx_own = k.tile(g, "idx_own", [128, 16], I32)
        idx_own2 = k.tile(g, "idx_own2", [128, 16], I32)
        idx_z = k.tile(g, "idx_z", [128, 32], I32)
        idx_ya = k.tile(g, "idx_ya", [128, 16], I32)
        idx_yb = k.tile(g, "idx_yb", [128, 16], I32)
        idx_aff = k.tile(g, "idx_aff", [128, 32], I32)
        gidt = k.tile(g, "gidt", [128, 32], F32)
        ybt = k.tile(g, "ybt", [128, 1], F32)
        epsb = k.tile(g, "epsb", [128, 1], F32)
        pidx = k.tile(g, "pidx", [128, 32, 8], I32)
        pk = k.tile(g, "pk", [128, 32, 8, 2], F32)
        es_zt = ExitStack()
        zt = k.tile(es_zt, "zt", [128, D], F32)

        k.dma("sp", cst.t[:], consts.t.ap(), w=[cst])
        for tl, src in ((idx_own, own_gid), (idx_own2, own_gid2), (idx_z, zrows), (idx_ya, yrowsA),
                        (idx_yb, yrowsB), (idx_aff, affrows), (gidt, gid_f), (ybt, ybase)):
            k.dma("sp", tl.t[:], src.t.ap(), w=[tl])
        k.op("dve", lambda e: e.tensor_copy(out=cbf.t[:, 0:128], in_=cst.t[:, 0:128]), r=[cst], w=[cbf])
        k.op("dve", lambda e: e.tensor_copy(out=cbf.t[:, 128:384], in_=cst.t[:, 128:384]), r=[cst], w=[cbf])
        k.op("pool", lambda e: e.memset(zt.t[:], 0.0), w=[zt])
        k.op("pool", lambda e: e.memset(epsb.t[:], EPS), w=[epsb])
        k.dma("pool", bsrc.t.ap(), zt.t[:, 0:128], r=[zt], w=[bsrc])
        for t in range(32):
            k.idma(out=yacc_sh.t[:, :], in_=zt.t[:, :], out_off=idx_z.t[:, t:t + 1], bounds=8191,
                   r=[zt, idx_z], w=[yacc_sh], sem="yz")

        with ExitStack() as es:
            cs_t = k.tile(es, "cs_t", [128, DC, 2], F32)
            cs_s = k.tile(es, "cs_s", [128, DC, 2], F32)
            wt = [k.tile(es, f"wada{i}", [128, DC, 512], F32) for i in range(2)]
            bt = k.tile(es, "bada", [2, 6 * D], F32)
            ada_sb = k.tile(es, "ada_sb", [2, 6 * D], F32)
            ps = [k.tile(es, f"adaps{i}", [128, 512], F32, psum=True) for i in range(2)]
            k.dma("sp", cs_t.t[:], cs.t.ap(), w=[cs_t])
            k.dma("sp", bt.t[:], bcast_rows(b_ada.t, 0, 6 * D, parts=2), w=[bt])
            k.op("act", lambda e: e.activation(out=cs_s.t[:], in_=cs_t.t[:], func=AF.Silu), r=[cs_t], w=[cs_s])
            wav = w_ada.t.ap().rearrange("(kc p) n -> p kc n", p=128)
            for cb in range(24):
                wtt = wt[cb % 2]
                pst = ps[cb % 2]
                k.dma("sp", wtt.t[:], wav[:, :, cb * 512:(cb + 1) * 512], w=[wtt])
                for kc in range(DC):
                    k.op("pe", lambda e: e.matmul(pst.t[0:2, :], lhsT=cs_s.t[:, kc, :], rhs=wtt.t[:, kc, :],
                                                  start=(kc == 0), stop=(kc == DC - 1)),
                         r=[cs_s, wtt], w=[pst])
                k.op("dve", lambda e: e.tensor_tensor(out=ada_sb.t[:, cb * 512:(cb + 1) * 512], in0=pst.t[0:2, :],
                                                      in1=bt.t[:, cb * 512:(cb + 1) * 512], op=ALU.add),
                     r=[pst, bt], w=[ada_sb])
            k.dma("sp", ada_d.t.ap(), ada_sb.t[:], r=[ada_sb], w=[ada_d])
        k.end_phase()
        es_zt.close()

        def load_bc(tl, src, off, n=D, eng="sp"):
            k.dma(eng, tl.t[:, 0:n], bcast_rows(src.t, off, n), w=[tl])

        with ExitStack() as es:
            A_t = k.tile(es, "A_t", [128, D], F32)
            B_t = k.tile(es, "B_t", [128, D], F32)
            nw = k.tile(es, "nw", [128, D], F32)
            gb = k.tile(es, "gb", [128, 16], F32)
            load_bc(nw, n1w, 0)
            load_bc(gb, gbias, 0, 16)

            def load_AB(row):
                load_bc(B_t, ada_d, row * 6 * D)
                load_bc(A_t, ada_d, row * 6 * D + D)
                k.op("dve", lambda e: e.scalar_tensor_tensor(out=A_t.t[:], in0=A_t.t[:], scalar=1.0, in1=nw.t[:],
                                                             op0=ALU.add, op1=ALU.mult), r=[A_t, nw], w=[A_t])
            xnT = k.tile(es, "xnT", [128, DC, NOWN], BF16)
            xt = [k.tile(es, f"xt{i}", [128, D], F32) for i in range(2)]
            tmpf = k.tile(es, "tmpf", [128, D], F32)
            junk = tmpf
            xnb = [k.tile(es, "xnb0", [128, D], BF16)]
            ss = k.tile(es, "ss", [128, 2], F32)
            tps = [k.tile(es, f"tps{i}", [128, 4, 128], BF16, psum=True) for i in range(2)]
            wts = [k.tile(es, f"wt{i}", [128, DC, 512], BF16) for i in range(2)]
            acc = [k.tile(es, f"acc{i}", [128, 512], F32, psum=True) for i in range(4)]
            stb = [k.tile(es, f"stb{i}", [128, 512], BF16) for i in range(3)]
            stf = [k.tile(es, f"stf{i}", [128, 512], F32) for i in range(3)]
            sig = [k.tile(es, f"sig{i}", [128, 512], F32) for i in range(2)]
            cnt = {"w": 0, "a": 0, "sb": 0, "sf": 0, "sg": 0, "ev": 0}
            wv = w_in.t.ap().rearrange("(dc p) n -> p dc n", p=128)

            def load_w(c0, n):
                t = wts[cnt["w"] % 2]
                cnt["w"] += 1
                k.dma("pool", t.t[:, :, 0:n], wv[:, :, c0:c0 + n], w=[t])
                return t

            def nxt(lst, key):
                t = lst[cnt[key] % len(lst)]
                cnt[key] += 1
                return t

            def build_xnT(src, row0, ntiles, A, B):
                for tt in range(ntiles):
                    x = xt[tt % 2]
                    k.dma("sp", x.t[:], src.t[row0 + tt * 128: row0 + (tt + 1) * 128, :], w=[x])
                    k.op("act", lambda e: e.activation(out=junk.t[:], in_=x.t[:], func=AF.Square,
                                                       accum_out=ss.t[:, 0:1]), r=[x], w=[junk, ss])
                    k.op("act", lambda e: e.activation(out=ss.t[:, 1:2], in_=ss.t[:, 0:1], func=AF.Sqrt, scale=1.0 / D, bias=epsb.t[:, 0:1]),
                         r=[ss, epsb], w=[ss])
                    k.op("dve", lambda e: e.reciprocal(out=ss.t[:, 1:2], in_=ss.t[:, 1:2]), r=[ss], w=[ss])
                    k.op("dve", lambda e: e.scalar_tensor_tensor(out=tmpf.t[:], in0=x.t[:], scalar=ss.t[:, 1:2],
                                                                 in1=A.t[:], op0=ALU.mult, op1=ALU.mult),
                         r=[x, ss, A], w=[tmpf])
                    xb = xnb[0]
                    k.op("dve", lambda e: e.tensor_tensor(out=xb.t[:], in0=tmpf.t[:], in1=B.t[:], op=ALU.add),
                         r=[tmpf, B], w=[xb])
                    for gq in range(4):
                        tp = tps[gq % 2]
                        for q in range(4):
                            dc = gq * 4 + q
                            k.op("pe", lambda e: e.transpose(tp.t[:, q, :], xb.t[:, dc * 128:(dc + 1) * 128], identb),
                                 r=[xb, cbf], w=[tp])
                        en = "act" if gq % 2 == 0 else "dve"
                        if en == "act":
                            k.op("act", lambda e: e.copy(out=xnT.t[:, gq * 4:(gq + 1) * 4, tt * 128:(tt + 1) * 128],
                                                         in_=tp.t[:]), r=[tp], w=[xnT])
                        else:
                            k.op("dve", lambda e: e.tensor_copy(out=xnT.t[:, gq * 4:(gq + 1) * 4, tt * 128:(tt + 1) * 128],
                                                                in_=tp.t[:]), r=[tp], w=[xnT])

            def tm_cols(wtile, ncols, ntiles, evac):
                for tt in range(ntiles):
                    a = nxt(acc, "a")
                    for dc in range(DC):
                        k.op("pe", lambda e: e.matmul(a.t[:, 0:ncols], lhsT=xnT.t[:, dc, tt * 128:(tt + 1) * 128],
                                                      rhs=wtile.t[:, dc, 0:ncols], start=(dc == 0), stop=(dc == DC - 1)),
                             r=[xnT, wtile], w=[a])
                    evac(a, tt)

            def fm_cols(wtile, ntok, evac, ncc=4):
                tb = min(512, ntok)
                for cc in range(ncc):
                    for b0 in range(0, ntok, tb):
                        a = nxt(acc, "a")
                        for dc in range(DC):
                            k.op("pe", lambda e: e.matmul(a.t[:, 0:tb], lhsT=wtile.t[:, dc, cc * 128:(cc + 1) * 128],
                                                          rhs=xnT.t[:, dc, b0:b0 + tb], start=(dc == 0), stop=(dc == DC - 1)),
                                 r=[xnT, wtile], w=[a])
                        evac(a, cc, b0, tb)

            def evac_engine():
                cnt["ev"] += 1
                return "act" if cnt["ev"] % 2 == 0 else "dve"

            def scaled_copy(en, o, i, scale, r, w):
                if en == "act":
                    k.op("act", lambda e: e.activation(out=o, in_=i, func=AF.Copy, scale=scale), r=r, w=w)
                else:
                    k.op("dve", lambda e: e.tensor_scalar(out=o, in0=i, scalar1=scale, scalar2=None, op0=ALU.mult),
                         r=r, w=w)

            def project(ntok, chunk0, full):
                ntiles = ntok // 128
                nchk = ntiles
                for half in range(2):
                    wtile = load_w(1024 + half * 512, 512)

                    def ev_kT(a, cc, b0, tb, half=half):
                        s = nxt(stb, "sb")
                        scaled_copy(evac_engine(), s.t[:, 0:tb], a.t[:, 0:tb], 1.0 / 16.0, [a], [s])
                        c0 = chunk0 + b0 // 128
                        hd = half * 4 + cc
                        k.dma("act", kT_d.t[c0:c0 + tb // 128, :, hd * 128:(hd + 1) * 128].rearrange("c p t -> p c t"),
                              s.t[:, 0:tb].rearrange("p (c t) -> p c t", t=128), r=[s], w=[kT_d])
                    fm_cols(wtile, ntok, ev_kT)

                    def ev_k(a, tt, half=half):
                        s = nxt(stb, "sb")
                        scaled_copy(evac_engine(), s.t[:], a.t[:], 1.0 / 16.0, [a], [s])
                        k.dma("act", k_d.t[(chunk0 + tt) * 128:(chunk0 + tt + 1) * 128, half * 512:(half + 1) * 512],
                              s.t[:], r=[s], w=[k_d])
                    tm_cols(wtile, 512, ntiles, ev_k)
                for half in range(2):
                    wtile = load_w(2048 + half * 512, 512)

                    def ev_v(a, tt, half=half):
                        s = nxt(stb, "sb")
                        scaled_copy(evac_engine(), s.t[:], a.t[:], 1.0, [a], [s])
                        k.dma("act", v_d.t[(chunk0 + tt) * 128:(chunk0 + tt + 1) * 128, half * 512:(half + 1) * 512],
                              s.t[:], r=[s], w=[v_d])
                    tm_cols(wtile, 512, ntiles, ev_v)
                wtile = load_w(4096, 16)

                def ev_g(a, tt):
                    s = nxt(stf, "sf")
                    k.op("dve", lambda e: e.tensor_tensor(out=s.t[:, 0:16], in0=a.t[:, 0:16], in1=gb.t[:], op=ALU.add),
                         r=[a, gb], w=[s])
                    k.dma("act", g_d.t[(chunk0 + tt) * 128:(chunk0 + tt + 1) * 128, :], s.t[:, 0:16], r=[s], w=[g_d])
                tm_cols(wtile, 16, ntiles, ev_g)
                if not full:
                    return
                for half in range(2):
                    wtile = load_w(half * 512, 512)

                    def ev_q(a, cc, b0, tb, half=half):
                        s = nxt(stb, "sb")
                        scaled_copy(evac_engine(), s.t[:, 0:tb], a.t[:, 0:tb], 1.0, [a], [s])
                        c0 = b0 // 128
                        hd = half * 4 + cc
                        k.dma("act", qT_d.t[c0:c0 + tb // 128, :, hd * 128:(hd + 1) * 128].rearrange("c p t -> p c t"),
                              s.t[:, 0:tb].rearrange("p (c t) -> p c t", t=128), r=[s], w=[qT_d])
                    fm_cols(wtile, ntok, ev_q)
                for half in range(2):
                    wtile = load_w(3072 + half * 512, 512)

                    def ev_o(a, tt, half=half):
                        s = nxt(stf, "sf")
                        scaled_copy(evac_engine(), s.t[:], a.t[:], 1.0, [a], [s])
                        k.dma("act", o_d.t[tt * 128:(tt + 1) * 128, half * 512:(half + 1) * 512], s.t[:], r=[s], w=[o_d])
                    tm_cols(wtile, 512, ntiles, ev_o)
                for half in range(2):
                    wa = load_w(4112 + half * 512, 512)
                    wgl = load_w(4112 + 1024 + half * 512, 512)
                    for cc in range(4):
                        for b0 in range(0, ntok, 512):
                            pa = nxt(acc, "a")
                            pg = nxt(acc, "a")
                            for (pp, ww) in ((pa, wa), (pg, wgl)):
                                for dc in range(DC):
                                    k.op("pe", lambda e: e.matmul(pp.t[:], lhsT=ww.t[:, dc, cc * 128:(cc + 1) * 128],
                                                                  rhs=xnT.t[:, dc, b0:b0 + 512], start=(dc == 0), stop=(dc == DC - 1)),
                                         r=[xnT, ww], w=[pp])
                            sg = nxt(sig, "sg")
                            k.op("act", lambda e: e.activation(out=sg.t[:], in_=pg.t[:], func=AF.Sigmoid), r=[pg], w=[sg])
                            s = nxt(stb, "sb")
                            k.op("dve", lambda e: e.tensor_tensor(out=s.t[:], in0=pa.t[:], in1=sg.t[:], op=ALU.mult),
                                 r=[pa, sg], w=[s])
                            k.dma("act", uT_d.t[half * 4 + cc, :, b0:b0 + 512], s.t[:], r=[s], w=[uT_d])

            load_AB(1)
            build_xnT(ctxl, 0, 2, A_t, B_t)
            project(NCTX, 0, False)
            load_AB(0)
            build_xnT(xall, 0, 16, A_t, B_t)
            project(NOWN, 2, True)
            build_xnT(xall, NOWN, 16, A_t, B_t)
            project(NOWN, 18, False)
        k.end_phase()

        for j in range(NPRE):
            for (src, dst) in ((wg, wgb), (wu, wub)):
                for q in range(4):
                    rs = slice(q * (D // 4), (q + 1) * (D // 4))
                    k.dma("pool", dst.t[j, rs, :].rearrange("d (a b) -> d a b", b=RUN),
                          src.t[j, rs, :].rearrange("d (a b) -> d a b", b=RUN), r=[], w=[dst], sem="pre")
            for q in range(4):
                fq = FF // 4
                k.dma("pool", wdb.t[j, q * fq:(q + 1) * fq, :], wd.t[j, q * fq:(q + 1) * fq, :], r=[], w=[wdb], sem="pre")
        if NPRE:
            k.keep.add("pre")
            k.nosync.add("pre")
            for T_ in (wgb, wub, wdb):
                T_.persist = True

        with ExitStack() as es:
            gall = k.tile(es, "gall", [128, NCH, 16], F32)
            lf = k.tile(es, "lf", [128, NCH, 16], F32)
            k.dma("sp", gall.t[:], g_d.t.ap().rearrange("(c p) n -> p c n", p=128), w=[gall])
            k.op("act", lambda e: e.activation(out=lf.t[:], in_=gall.t[:], func=AF.Exp, scale=-1.0), r=[gall], w=[lf])
            k.op("act", lambda e: e.activation(out=lf.t[:], in_=lf.t[:], func=AF.Ln, bias=1.0), r=[lf], w=[lf])
            k.op("dve", lambda e: e.tensor_scalar(out=lf.t[:], in0=lf.t[:], scalar1=-1.0, scalar2=None, op0=ALU.mult),
                 r=[lf], w=[lf])
            NV = NCH * 4
            es_pro = ExitStack()
            pp1 = k.tile(es_pro, "pp1", [128, 512], F32, psum=True)
            pp2 = k.tile(es_pro, "pp2", [128, 512], F32, psum=True)
            wcol = [k.tile(es, f"wcol{d}", [128, NCH, 4], F32) for d in range(2)]
            wint = [k.tile(es, f"wint{d}", [128, NCH, 4], F32) for d in range(2)]
            clmp = [k.tile(es, f"clmp{d}", [128, NCH, 4], F32) for d in range(2)]
            acol = k.tile(es, "acol", [128, NCH, 4], F32)
            bcol = k.tile(es, "bcol", [128, NCH, 4], F32)
            grep = k.tile(es, "grep", [128, NCH, 4], F32)
            mmax = k.tile(es, "mmax", [128, NCH, 4], F32)
            Mp = k.tile(es, "Mp", [128, NCH, 4], F32)
            warg = k.tile(es, "warg", [128, NCH, 4], F32)
            mcur = k.tile(es, "mcur", [128, 4], F32)
            aT = k.tile(es, "aT", [128, 2, 128], F32)
            mx = k.tile(es, "mx", [128, 2], F32)
            dg = k.tile(es, "dg", [128, 136], F32)
            orders = [list(range(0, 18)), [1, 0] + list(range(33, 17, -1)) + list(range(17, 1, -1))]
            for d in range(2):
                tri = triF if d == 0 else triB
                i0, f0 = d * 8, d * 8 + 4
                k.op("pe", lambda e: e.matmul(pp1.t[:, 0:NV].rearrange("p (c h) -> p c h", h=4), lhsT=tri,
                                              rhs=lf.t[:, :, f0:f0 + 4], start=True, stop=True), r=[cst, lf], w=[pp1])
                k.op("dve", lambda e: e.tensor_copy(out=bcol.t[:], in_=pp1.t[:, 0:NV].rearrange("p (c h) -> p c h", h=4)),
                     r=[pp1], w=[bcol])
                k.op("dve", lambda e: e.tensor_tensor(out=acol.t[:], in0=gall.t[:, :, i0:i0 + 4], in1=bcol.t[:], op=ALU.subtract),
                     r=[gall, bcol], w=[acol])
                k.op("pe", lambda e: e.matmul(pp2.t[:, 0:NV].rearrange("p (c h) -> p c h", h=4), lhsT=ones,
                                              rhs=lf.t[:, :, f0:f0 + 4], start=True, stop=True), r=[cst, lf], w=[pp2])
                k.op("dve", lambda e: e.tensor_copy(out=grep.t[:], in_=pp2.t[:, 0:NV].rearrange("p (c h) -> p c h", h=4)),
                     r=[pp2], w=[grep])
                af = acol.t[:].rearrange("p c h -> p (c h)")
                k.op("pe", lambda e: e.transpose(pp1.t[:, 0:128], af[:, 0:128], ident), r=[acol, cst], w=[pp1])
                k.op("pe", lambda e: e.transpose(pp1.t[0:8, 128:256], af[:, 128:136], ident), r=[acol, cst], w=[pp1])
                k.op("dve", lambda e: e.reduce_max(out=mx.t[:, 0:1], in_=pp1.t[:, 0:128], axis=AX.X), r=[pp1], w=[mx])
                k.op("dve", lambda e: e.reduce_max(out=mx.t[0:8, 1:2], in_=pp1.t[0:8, 128:256], axis=AX.X), r=[pp1], w=[mx])
                k.op("dve", lambda e: e.tensor_scalar(out=dg.t[:, 0:128], in0=ident, scalar1=mx.t[:, 0:1], scalar2=None,
                                                      op0=ALU.mult), r=[cst, mx], w=[dg])
                k.op("dve", lambda e: e.tensor_scalar(out=dg.t[0:8, 128:136], in0=cst.t[0:8, 0:8], scalar1=mx.t[0:8, 1:2],
                                                      scalar2=None, op0=ALU.mult), r=[cst, mx], w=[dg])
                k.op("pe", lambda e: e.matmul(pp2.t[:, 0:128], lhsT=ones, rhs=dg.t[:, 0:128], start=True, stop=True),
                     r=[cst, dg], w=[pp2])
                k.op("pe", lambda e: e.matmul(pp2.t[:, 128:136], lhsT=cst.t[0:8, 384:512], rhs=dg.t[0:8, 128:136],
                                              start=True, stop=True), r=[cst, dg], w=[pp2])
                k.op("dve", lambda e: e.tensor_copy(out=mmax.t[:].rearrange("p c h -> p (c h)"), in_=pp2.t[:, 0:NV]),
                     r=[pp2], w=[mmax])
                k.op("dve", lambda e: e.memset(mcur.t[:], 0.0), w=[mcur])
                for c in orders[d]:
                    k.op("dve", lambda e: e.tensor_tensor(out=Mp.t[:, c, :], in0=mmax.t[:, c, :], in1=mcur.t[:], op=ALU.max),
                         r=[mmax, mcur], w=[Mp])
                    k.op("dve", lambda e: e.tensor_tensor(out=warg.t[:, c, :], in0=mcur.t[:], in1=Mp.t[:, c, :], op=ALU.subtract),
                         r=[mcur, Mp], w=[warg])
                    k.op("dve", lambda e: e.tensor_tensor(out=mcur.t[:], in0=grep.t[:, c, :], in1=Mp.t[:, c, :], op=ALU.add),
                         r=[grep, Mp], w=[mcur])
                k.op("dve", lambda e: e.tensor_tensor(out=acol.t[:], in0=acol.t[:], in1=Mp.t[:], op=ALU.subtract),
                     r=[acol, Mp], w=[acol])
                k.op("act", lambda e: e.activation(out=wcol[d].t[:], in_=acol.t[:], func=AF.Exp), r=[acol], w=[wcol[d]])
                k.op("act", lambda e: e.activation(out=wint[d].t[:], in_=warg.t[:], func=AF.Exp), r=[warg], w=[wint[d]])
                k.op("dve", lambda e: e.tensor_tensor(out=bcol.t[:], in0=bcol.t[:], in1=Mp.t[:], op=ALU.add),
                     r=[bcol, Mp], w=[bcol])
                k.op("act", lambda e: e.activation(out=clmp[d].t[:], in_=bcol.t[:], func=AF.Exp, scale=-1.0),
                     r=[bcol], w=[clmp[d]])

            k.sync_all()
            es_pro.close()
            CxD = [[k.tile(es, f"Cx{d}_{h}", [128, 2, 257], F32) for h in range(NH)] for d in range(2)]
            Cb = [k.tile(es, f"Cb{h}", [128, 2, 257], BF16) for h in range(NH)]
            kt = [k.tile(es, f"kt{i}", [128, 8, 128], BF16) for i in range(2)]
            qt = [k.tile(es, f"qt{i}", [128, 8, 128], BF16) for i in range(2)]
            kk = [k.tile(es, f"kk{i}", [128, W], BF16) for i in range(2)]
            vv = [k.tile(es, f"vv{i}", [128, NH, 257], BF16) for i in range(2)]
            vx = [k.tile(es, f"vx{i}", [128, 257], BF16) for i in range(2)]
            sT = [k.tile(es, f"sT{i}", [128, 128], BF16) for i in range(2)]
            STp1 = k.tile(es, "STp", [128, 2, 128], F32, psum=True)
            STp = [STp1, STp1]
            Pp = [k.tile(es, f"Pp{i}", [128, 257], F32, psum=True) for i in range(2)]
            Cnps = [k.tile(es, f"Cnp{i}", [128, 2, 512], F32, psum=True) for i in range(2)]
            dens = [k.tile(es, f"den{i}", [128, 2], F32) for i in range(NH)]
            hh = [k.tile(es, f"hh{i}", [128, W], F32) for i in range(2)]
            h1 = k.tile(es, "h1", [128, W], F32)
            oo = k.tile(es, "oo", [128, W], F32)
            mw = k.tile(es, "mw", [128, W], F32)
            st6s = [k.tile(es, f"st6_{i}", [128, 6], F32) for i in range(NH)]
            mvs = [k.tile(es, f"mv{i}", [128, 2], F32) for i in range(NH)]
            mlb = k.tile(es, "mlb", [128, W], BF16)
            mst = k.tile(es, "mst", [128, 8, 128], BF16)
            tp2 = k.tile(es, "tp2", [128, 4, 128], BF16, psum=True)
            load_bc(mw, mnw, 0, W)
            for i in range(2):
                k.op("pool", lambda e: e.memset(vv[i].t[:, :, 256:257], 1.0), w=[vv[i]])
            step = 0
            for d in range(2):
                for h in range(NH):
                    k.op("pool", lambda e: e.memset(CxD[d][h].t[:], 0.0), w=[CxD[d][h]])
            sched = []
            for n_ in range(18):
                sched.append((0, orders[0][n_]))
                sched.append((1, orders[1][n_]))
            sched += [(1, c_) for c_ in orders[1][18:]]
            synced = False
            for (d, c) in sched:
                if True:
                    maskb = maskFb if d == 0 else maskBb
                    Cx = CxD[d]
                    full = 2 <= c < 18
                    if d == 1 and full and not synced:
                        synced = True
                        k.sync_all()
                    i = step % 2
                    step += 1
                    k.dma("sp", kt[i].t[:], kT_d.t[c].rearrange("p (a t) -> p a t", t=128), w=[kt[i]])
                    k.dma("sp", kk[i].t[:], k_d.t[c * 128:(c + 1) * 128, :], w=[kk[i]])
                    k.dma("sp", vv[i].t[:, :, 0:256], v_d.t[c * 128:(c + 1) * 128, :].rearrange("p (h e) -> p h e", e=256),
                          w=[vv[i]])
                    if full:
                        k.dma("sp", qt[i].t[:], qT_d.t[c - 2].rearrange("p (a t) -> p a t", t=128), w=[qt[i]])
                        if d == 1:
                            k.dma("sp", h1.t[:], h1_d.t[(c - 2) * 128:(c - 1) * 128, :], w=[h1])
                            k.dma("sp", oo.t[:], o_d.t[(c - 2) * 128:(c - 1) * 128, :], w=[oo])
                    hcur = hh[i]
                    for h in range(NH):
                        wc = wcol[d].t[:, c, h:h + 1]
                        wi = wint[d].t[:, c, h:h + 1]
                        cl = clmp[d].t[:, c, h:h + 1]
                        vxt = vx[h % 2]
                        den = dens[h]
                        Cnp = Cnps[h % 2]
                        k.op("act", lambda e: e.activation(out=vxt.t[:], in_=vv[i].t[:, h, :], func=AF.Copy, scale=wc),
                             r=[vv[i], wcol[d]], w=[vxt])
                        if full:
                            k.op("act", lambda e: e.activation(out=Cb[h].t[:], in_=Cx[h].t[:], func=AF.Copy, scale=wi),
                                 r=[Cx[h], wint[d]], w=[Cb[h]])
                            sp_ = STp[h % 2]
                            for dc in range(2):
                                k.op("pe", lambda e: e.matmul(sp_.t[:, h % 2, :], lhsT=kt[i].t[:, h * 2 + dc, :], rhs=qt[i].t[:, h * 2 + dc, :],
                                                              start=(dc == 0), stop=(dc == 1)), r=[kt[i], qt[i]], w=[sp_])
                            s_ = sT[h % 2]
                            k.op("dve", lambda e: e.tensor_tensor(out=s_.t[:], in0=sp_.t[:, h % 2, :], in1=maskb, op=ALU.mult),
                                 r=[sp_, cbf], w=[s_])
                            p_ = Pp[h % 2]
                            k.op("pe", lambda e: e.matmul(p_.t[:], lhsT=s_.t[:], rhs=vxt.t[:], start=True, stop=False),
                                 r=[s_, vxt], w=[p_])
                            for dc in range(2):
                                k.op("pe", lambda e: e.matmul(p_.t[:], lhsT=qt[i].t[:, h * 2 + dc, :], rhs=Cb[h].t[:, dc, :],
                                                              start=False, stop=(dc == 1)), r=[qt[i], Cb[h]], w=[p_])
                            k.op("act", lambda e: e.activation(out=den.t[:, 0:1], in_=p_.t[:, 256:257], func=AF.Abs), r=[p_], w=[den])
                            k.op("dve", lambda e: e.tensor_scalar(out=den.t[:, 0:1], in0=den.t[:, 0:1], scalar1=cl, scalar2=None,
                                                                  op0=ALU.max), r=[den, clmp[d]], w=[den])
                            k.op("dve", lambda e: e.reciprocal(out=den.t[:, 1:2], in_=den.t[:, 0:1]), r=[den], w=[den])
                            k.op("act", lambda e: e.activation(out=hcur.t[:, h * 256:(h + 1) * 256], in_=p_.t[:, 0:256],
                                                               func=AF.Copy, scale=den.t[:, 1:2]), r=[p_, den], w=[hcur])
                        for dc in range(2):
                            k.op("pe", lambda e: e.matmul(Cnp.t[:, dc, 0:257], lhsT=kk[i].t[:, h * 256 + dc * 128: h * 256 + (dc + 1) * 128],
                                                          rhs=vxt.t[:], start=True, stop=True), r=[kk[i], vxt], w=[Cnp])
                        k.op("dve", lambda e: e.scalar_tensor_tensor(out=Cx[h].t[:], in0=Cx[h].t[:], scalar=wi, in1=Cnp.t[:, :, 0:257],
                                                                     op0=ALU.mult, op1=ALU.add), r=[Cx[h], wint[d], Cnp], w=[Cx[h]])
                    if not full:
                        continue
                    r0 = (c - 2) * 128
                    if d == 0:
                        k.dma("act", h1_d.t[r0:r0 + 128, :], hcur.t[:], r=[hcur], w=[h1_d])
                        continue
                    k.op("dve", lambda e: e.tensor_tensor(out=hcur.t[:], in0=hcur.t[:], in1=h1.t[:], op=ALU.add),
                         r=[hcur, h1], w=[hcur])
                    k.op("act", lambda e: e.activation(out=oo.t[:], in_=oo.t[:], func=AF.Sigmoid), r=[oo], w=[oo])
                    for h in range(NH):
                        hs = hcur.t[:, h * 256:(h + 1) * 256]
                        st6 = st6s[h]
                        mv = mvs[h]
                        k.op("dve", lambda e: e.bn_stats(out=st6.t[:], in_=hs), r=[hcur], w=[st6])
                        k.op("dve", lambda e: e.bn_aggr(out=mv.t[:], in_=st6.t[:]), r=[st6], w=[mv])
                        k.op("act", lambda e: e.activation(out=mv.t[:, 1:2], in_=mv.t[:, 1:2], func=AF.Sqrt, bias=epsb.t[:, 0:1]),
                             r=[mv, epsb], w=[mv])
                        k.op("dve", lambda e: e.reciprocal(out=mv.t[:, 1:2], in_=mv.t[:, 1:2]), r=[mv], w=[mv])
                        k.op("dve", lambda e: e.tensor_scalar(out=hs, in0=hs, scalar1=mv.t[:, 0:1], scalar2=mv.t[:, 1:2],
                                                              op0=ALU.subtract, op1=ALU.mult), r=[hcur, mv], w=[hcur])
                    k.op("dve", lambda e: e.tensor_tensor(out=hcur.t[:], in0=hcur.t[:], in1=oo.t[:], op=ALU.mult),
                         r=[hcur, oo], w=[hcur])
                    k.op("dve", lambda e: e.tensor_tensor(out=mlb.t[:], in0=hcur.t[:], in1=mw.t[:], op=ALU.mult),
                         r=[hcur, mw], w=[mlb])
                    for gq in range(2):
                        for q in range(4):
                            fc = gq * 4 + q
                            k.op("pe", lambda e: e.transpose(tp2.t[:, q, :], mlb.t[:, fc * 128:(fc + 1) * 128], identb),
                                 r=[mlb, cbf], w=[tp2])
                        k.op("act", lambda e: e.copy(out=mst.t[:, gq * 4:(gq + 1) * 4, :], in_=tp2.t[:]), r=[tp2], w=[mst])
                    k.dma("act", mixT_d.t[0:8, :, r0:r0 + 128].rearrange("f p t -> p f t"), mst.t[:], r=[mst], w=[mixT_d])
        k.end_phase()

        with ExitStack() as es:
            cw = k.tile(es, "cw", [128, 8, KT], F32)
            cbv = k.tile(es, "cbv", [128, 8], F32)
            lw = k.tile(es, "lw", [128, 8], F32)
            lb = k.tile(es, "lb", [128, 8], F32)
            for tl, src in ((cw, convw), (cbv, convb), (lw, cnw), (lb, cnb)):
                k.dma("sp", tl.t[:], src.t.ap(), w=[tl])
            TB = 1024
            dgs = k.tile(es, "dgs", [128, 8 * KT, 128], BF16)
            for cc in range(8):
                for t in range(KT):
                    k.op("dve", lambda e: e.tensor_scalar(out=dgs.t[:, cc * KT + t, :], in0=identb, scalar1=cw.t[:, cc, t:t + 1],
                                                          scalar2=None, op0=ALU.mult), r=[cbf, cw], w=[dgs])
            u = k.tile(es, "u", [128, 8, TB], BF16)
            ycs = [k.tile(es, f"yc{cc}", [128, TB], F32) for cc in range(8)]
            sq = k.tile(es, "sq", [128, TB], F32)
            yps = [k.tile(es, f"yps{i}", [128, TB], F32, psum=True) for i in range(2)]
            sps = [k.tile(es, f"sps{i}", [128, 512], F32, psum=True) for i in range(4)]
            mean = k.tile(es, "mean", [128, TB], F32)
            rstd = k.tile(es, "rstd", [128, TB], F32)
            mo = [k.tile(es, f"mo{i}", [128, TB], BF16) for i in range(2)]
            taps = [15] + [t for t in range(KT) if t != 15]
            for blk in range(NOWN // TB):
                t0 = blk * TB
                k.dma("sp", u.t[:], uT_d.t[:, :, t0:t0 + TB].rearrange("c p t -> p c t"), w=[u])
                for cc in range(8):
                    y = ycs[cc]
                    yp = yps[cc % 2]
                    for hf in range(TB // 512):
                        y3 = yp.t[:, hf * 512:(hf + 1) * 512].rearrange("p (r t) -> p r t", t=64)
                        u3 = u.t[:, cc, hf * 512:(hf + 1) * 512].rearrange("p (r t) -> p r t", t=64)
                        for ti, t in enumerate(taps):
                            o = t - 15
                            lo, hi = max(0, -o), 64 - max(0, o)
                            k.op("pe", lambda e: e.matmul(y3[:, :, lo:hi], lhsT=dgs.t[:, cc * KT + t, :], rhs=u3[:, :, lo + o:hi + o],
                                                          start=(ti == 0), stop=(ti == KT - 1), skip_group_check=True),
                                 r=[dgs, u], w=[yp])
                    k.op("act", lambda e: e.activation(out=y.t[:], in_=yp.t[:], func=AF.Identity, bias=cbv.t[:, cc:cc + 1]),
                         r=[yp, cbv], w=[y])
                for cc in range(8):
                    y = ycs[cc]
                    k.op("act", lambda e: e.activation(out=sq.t[:], in_=y.t[:], func=AF.Square), r=[y], w=[sq])
                    for hf in range(2):
                        k.op("pe", lambda e: e.matmul(sps[hf].t[:], lhsT=ones, rhs=y.t[:, hf * 512:(hf + 1) * 512],
                                                      start=(cc == 0), stop=(cc == 7)), r=[cst, y], w=[sps[hf]])
                        k.op("pe", lambda e: e.matmul(sps[2 + hf].t[:], lhsT=ones, rhs=sq.t[:, hf * 512:(hf + 1) * 512],
                                                      start=(cc == 0), stop=(cc == 7)), r=[cst, sq], w=[sps[2 + hf]])
                for hf in range(2):
                    sl = slice(hf * 512, (hf + 1) * 512)
                    k.op("dve", lambda e: e.tensor_scalar(out=mean.t[:, sl], in0=sps[hf].t[:], scalar1=1.0 / CW, scalar2=None,
                                                          op0=ALU.mult), r=[sps[hf]], w=[mean])
                    k.op("dve", lambda e: e.tensor_tensor(out=sq.t[:, sl], in0=mean.t[:, sl], in1=mean.t[:, sl], op=ALU.mult),
                         r=[mean], w=[sq])
                    k.op("dve", lambda e: e.scalar_tensor_tensor(out=rstd.t[:, sl], in0=sps[2 + hf].t[:], scalar=1.0 / CW,
                                                                 in1=sq.t[:, sl], op0=ALU.mult, op1=ALU.subtract),
                         r=[sps[2 + hf], sq], w=[rstd])
                    k.op("act", lambda e: e.activation(out=rstd.t[:, sl], in_=rstd.t[:, sl], func=AF.Sqrt, bias=epsb.t[:, 0:1]),
                         r=[rstd, epsb], w=[rstd])
                    k.op("dve", lambda e: e.reciprocal(out=rstd.t[:, sl], in_=rstd.t[:, sl]), r=[rstd], w=[rstd])
                for cc in range(8):
                    y = ycs[cc]
                    en = "dve" if cc % 2 == 0 else "pool"
                    k.op(en, lambda e: e.tensor_tensor(out=y.t[:], in0=y.t[:], in1=mean.t[:], op=ALU.subtract),
                         r=[y, mean], w=[y])
                    k.op(en, lambda e: e.tensor_tensor(out=y.t[:], in0=y.t[:], in1=rstd.t[:], op=ALU.mult),
                         r=[y, rstd], w=[y])
                    m = mo[cc % 2]
                    k.op("act", lambda e: e.activation(out=m.t[:], in_=y.t[:], func=AF.Silu, bias=lb.t[:, cc:cc + 1],
                                                       scale=lw.t[:, cc:cc + 1]), r=[y, lw, lb], w=[m])
                    k.dma("act", mixT_d.t[8 + cc, :, t0:t0 + TB], m.t[:], r=[m], w=[mixT_d])
        k.end_phase()

        with ExitStack() as es:
            A2 = k.tile(es, "A2", [128, D], F32)
            B2 = k.tile(es, "B2", [128, D], F32)
            wo = k.tile(es, "wo", [128, DC, D], BF16)
            wr = k.tile(es, "wr", [128, DC, 16], F32)
            es_pre = ExitStack()
            g1b = k.tile(es_pre, "g1b", [128, D], F32)
            nw2 = k.tile(es_pre, "nw2", [128, D], F32)
            wof = [k.tile(es_pre, f"wof{i}", [128, D], F32) for i in range(2)]
            load_bc(g1b, ada_d, 2 * D)
            load_bc(B2, ada_d, 3 * D)
            load_bc(A2, ada_d, 4 * D)
            load_bc(nw2, n2w, 0)
            k.dma("sp", wr.t[:], w_r.t.ap(), w=[wr])
            k.op("dve", lambda e: e.scalar_tensor_tensor(out=A2.t[:], in0=A2.t[:], scalar=1.0, in1=nw2.t[:],
                                                         op0=ALU.add, op1=ALU.mult), r=[A2, nw2], w=[A2])
            for fc in range(DC):
                wf = wof[fc % 2]
                k.dma("sp", wf.t[:], w_out.t[fc * 128:(fc + 1) * 128, :], w=[wf])
                k.op("dve", lambda e: e.tensor_tensor(out=wo.t[:, fc, :], in0=wf.t[:], in1=g1b.t[:], op=ALU.mult),
                     r=[wf, g1b], w=[wo])
            k.sync_all()
            es_pre.close()
            mt = [k.tile(es, f"mt{i}", [128, DC, 512], BF16) for i in range(2)]
            xt = [k.tile(es, f"x4_{i}", [128, D], F32) for i in range(2)]
            x1 = [k.tile(es, f"x1_{i}", [128, D], F32) for i in range(2)]
            junk = k.tile(es, "junk4", [128, D], BF16)
            xn2 = k.tile(es, "xn2", [128, D], F32)
            xn2b = [k.tile(es, f"xn2b{i}", [128, D], BF16) for i in range(2)]
            xT = k.tile(es, "xT", [128, DC, 128], F32)
            ss = k.tile(es, "ss4", [128, 2], F32)
            pacc = [k.tile(es, f"pacc{i}", [128, 512], F32, psum=True) for i in range(3)]
            ptr = [k.tile(es, f"ptr{i}", [128, 4, 128], F32, psum=True) for i in range(2)]
            plg = k.tile(es, "plg", [128, 16], F32, psum=True)
            sm = k.tile(es, "sm", [128, 4], F32)
            ex = k.tile(es, "ex", [128, 16], F32)
            afft = [k.tile(es, f"afft{i}", [128, 32], F32) for i in range(2)]
            na = 0
            for tb in range(NOWN // 512):
                m = mt[tb % 2]
                k.dma("sp", m.t[:], mixT_d.t[:, :, tb * 512:(tb + 1) * 512].rearrange("f p t -> p f t"), w=[m])
                for t4 in range(4):
                    tt = tb * 4 + t4
                    x = xt[tt % 2]
                    x1t = x1[tt % 2]
                    k.dma("sp", x.t[:], xall.t[tt * 128:(tt + 1) * 128, :], w=[x])
                    for nb in range(4):
                        a = pacc[na % 3]
                        na += 1
                        for fc in range(DC):
                            k.op("pe", lambda e: e.matmul(a.t[:], lhsT=m.t[:, fc, t4 * 128:(t4 + 1) * 128],
                                                          rhs=wo.t[:, fc, nb * 512:(nb + 1) * 512], start=(fc == 0), stop=(fc == DC - 1)),
                                 r=[m, wo], w=[a])
                        k.op("dve", lambda e: e.tensor_tensor(out=x1t.t[:, nb * 512:(nb + 1) * 512], in0=a.t[:],
                                                              in1=x.t[:, nb * 512:(nb + 1) * 512], op=ALU.add), r=[a, x], w=[x1t])
                    k.dma("act", x1_d.t[tt * 128:(tt + 1) * 128, :], x1t.t[:], r=[x1t], w=[x1_d])
                    k.op("act", lambda e: e.activation(out=junk.t[:], in_=x1t.t[:], func=AF.Square, accum_out=ss.t[:, 0:1]),
                         r=[x1t], w=[junk, ss])
                    k.op("act", lambda e: e.activation(out=ss.t[:, 1:2], in_=ss.t[:, 0:1], func=AF.Sqrt, scale=1.0 / D, bias=epsb.t[:, 0:1]),
                         r=[ss, epsb], w=[ss])
                    k.op("dve", lambda e: e.reciprocal(out=ss.t[:, 1:2], in_=ss.t[:, 1:2]), r=[ss], w=[ss])
                    k.op("dve", lambda e: e.scalar_tensor_tensor(out=xn2.t[:], in0=x1t.t[:], scalar=ss.t[:, 1:2], in1=A2.t[:],
                                                                 op0=ALU.mult, op1=ALU.mult), r=[x1t, ss, A2], w=[xn2])
                    k.op("dve", lambda e: e.tensor_tensor(out=xn2.t[:], in0=xn2.t[:], in1=B2.t[:], op=ALU.add),
                         r=[xn2, B2], w=[xn2])
                    xb = xn2b[tt % 2]
                    k.op("act", lambda e: e.copy(out=xb.t[:], in_=xn2.t[:]), r=[xn2], w=[xb])
                    k.idma(out=xn2_sh.t[:, :], in_=xb.t[:, :], out_off=idx_own.t[:, tt:tt + 1], bounds=4095,
                           r=[xb, idx_own], w=[xn2_sh])
                    for gq in range(4):
                        tp = ptr[gq % 2]
                        for q in range(4):
                            dc = gq * 4 + q
                            k.op("pe", lambda e: e.transpose(tp.t[:, q, :], xn2.t[:, dc * 128:(dc + 1) * 128], ident),
                                 r=[xn2, cst], w=[tp])
                        if gq % 2 == 0:
                            k.op("act", lambda e: e.copy(out=xT.t[:, gq * 4:(gq + 1) * 4, :], in_=tp.t[:]), r=[tp], w=[xT])
                        else:
                            k.op("dve", lambda e: e.tensor_copy(out=xT.t[:, gq * 4:(gq + 1) * 4, :], in_=tp.t[:]), r=[tp], w=[xT])
                    for dc in range(DC):
                        k.op("pe", lambda e: e.matmul(plg.t[:], lhsT=xT.t[:, dc, :], rhs=wr.t[:, dc, :], start=(dc == 0),
                                                      stop=(dc == DC - 1)), r=[xT, wr], w=[plg])
                    k.op("dve", lambda e: e.reduce_max(out=sm.t[:, 0:1], in_=plg.t[:], axis=AX.X), r=[plg], w=[sm])
                    k.op("dve", lambda e: e.tensor_scalar(out=sm.t[:, 1:2], in0=sm.t[:, 0:1], scalar1=-1.0, scalar2=None,
                                                          op0=ALU.mult), r=[sm], w=[sm])
                    k.op("act", lambda e: e.activation(out=ex.t[:], in_=plg.t[:], func=AF.Exp, bias=sm.t[:, 1:2],
                                                       accum_out=sm.t[:, 2:3]), r=[plg, sm], w=[ex, sm])
                    k.op("dve", lambda e: e.reciprocal(out=sm.t[:, 3:4], in_=sm.t[:, 2:3]), r=[sm], w=[sm])
                    af = afft[tt % 2]
                    k.op("dve", lambda e: e.tensor_scalar(out=af.t[:, 0:16], in0=ex.t[:], scalar1=sm.t[:, 3:4], scalar2=None,
                                                          op0=ALU.mult), r=[ex, sm], w=[af])
                    k.op("dve", lambda e: e.tensor_copy(out=af.t[:, 16:24], in_=af.t[:, 8:16]), r=[af], w=[af])
                    k.op("dve", lambda e: e.tensor_copy(out=af.t[:, 24:32], in_=af.t[:, 0:8]), r=[af], w=[af])
                    k.idma(out=aff_sh.t[:, :], in_=af.t[:, 0:16], out_off=idx_own.t[:, tt:tt + 1], bounds=8191,
                           r=[af, idx_own], w=[aff_sh])
                    k.idma(out=aff_sh.t[:, :], in_=af.t[:, 16:32], out_off=idx_own2.t[:, tt:tt + 1], bounds=8191,
                           r=[af, idx_own2], w=[aff_sh])
        k.pair_barrier(bsrc, bdst)
        if dbg:
            k.dma("sp", dbg_x1.t.ap(), x1_d.t.ap(), r=[x1_d], w=[dbg_x1])
            k.dma("sp", dbg_mix.t.ap(), mixT_d.t.ap(), r=[mixT_d], w=[dbg_mix])
            k.dma("sp", dbg_aff.t.ap(), aff_sh.t.ap(), r=[aff_sh], w=[dbg_aff])

        def tok_scatter(j, t):
            k.idma(out=tokg_d[j].t[:, :], in_=pk.t[:, t, j, :], out_off=pidx.t[:, t, j:j + 1], bounds=CAP - 1,
                   r=[pk, pidx], w=[tokg_d[j]], sem=f"tk{j}")

        with ExitStack() as es:
            affa = k.tile(es, "affa", [128, 32, 16], F32)
            affo = k.tile(es, "affo", [128, 32, 8], F32)
            for t in range(32):
                k.idma(out=affa.t[:, t, :], in_=aff_sh.t[:, :], in_off=idx_aff.t[:, t:t + 1], bounds=8191,
                       r=[aff_sh, idx_aff], w=[affa])
            k.op("dve", lambda e: e.tensor_copy(out=affo.t[:], in_=affa.t[:, :, 0:8]), r=[affa], w=[affo])
            lo = k.tile(es, "lo", [128, 8], F32)
            mid = k.tile(es, "mid", [128, 8], F32)
            cntt = k.tile(es, "cntt", [128, 8], F32)
            stp = k.tile(es, "stp", [128, 8], F32)
            cmp = k.tile(es, "cmp", [128, 32], F32)
            tot = k.tile(es, "tot", [128, 8], F32, psum=True)
            k.op("dve", lambda e: e.memset(lo.t[:], 0.0), w=[lo])
            for it in range(1, NBIS + 1):
                dk = 2.0 ** (-it)
                k.op("dve", lambda e: e.tensor_scalar(out=mid.t[:], in0=lo.t[:], scalar1=dk, scalar2=None, op0=ALU.add),
                     r=[lo], w=[mid])
                for j in range(EL):
                    k.op("dve", lambda e: e.tensor_scalar(out=cmp.t[:], in0=affo.t[:, :, j], scalar1=mid.t[:, j:j + 1], scalar2=0.0,
                                                          op0=ALU.is_ge, op1=ALU.add, accum_out=cntt.t[:, j:j + 1]),
                         r=[affo, mid], w=[cmp, cntt])
                k.op("pe", lambda e: e.matmul(tot.t[:], lhsT=ones, rhs=cntt.t[:], start=True, stop=True), r=[cst, cntt], w=[tot])
                k.op("dve", lambda e: e.tensor_scalar(out=stp.t[:], in0=tot.t[:], scalar1=CAP - 0.5, scalar2=dk,
                                                      op0=ALU.is_ge, op1=ALU.mult), r=[tot], w=[stp])
                k.op("dve", lambda e: e.tensor_tensor(out=lo.t[:], in0=lo.t[:], in1=stp.t[:], op=ALU.add), r=[lo, stp], w=[lo])
            mask = k.tile(es, "mask", [128, 32, 8], F32)
            for j in range(EL):
                k.op("dve", lambda e: e.tensor_scalar(out=mask.t[:, :, j], in0=affo.t[:, :, j], scalar1=lo.t[:, j:j + 1], scalar2=None,
                                                      op0=ALU.is_ge), r=[affo, lo], w=[mask])
            ppos = k.tile(es, "ppos", [128, 256], F32, psum=True)
            pcnt = k.tile(es, "pcnt", [128, 256], F32, psum=True)
            mflat = mask.t[:].rearrange("p t j -> p (t j)")
            k.op("pe", lambda e: e.matmul(ppos.t[:], lhsT=strictL, rhs=mflat, start=True, stop=True), r=[cst, mask], w=[ppos])
            k.op("pe", lambda e: e.matmul(pcnt.t[:], lhsT=ones, rhs=mflat, start=True, stop=True), r=[cst, mask], w=[pcnt])
            sA = k.tile(es, "sA", [128, 32, 8], F32)
            sB = k.tile(es, "sB", [128, 32, 8], F32)
            cn0 = k.tile(es, "cn0", [128, 32, 8], F32)
            k.op("dve", lambda e: e.tensor_copy(out=cn0.t[:].rearrange("p t j -> p (t j)"), in_=pcnt.t[:]), r=[pcnt], w=[cn0])
            k.op("dve", lambda e: e.tensor_copy(out=sA.t[:], in_=cn0.t[:]), r=[cn0], w=[sA])
            a_, b_ = sA, sB
            for s in (1, 2, 4, 8, 16):
                k.op("dve", lambda e: e.tensor_copy(out=b_.t[:], in_=a_.t[:]), r=[a_], w=[b_])
                k.op("dve", lambda e: e.tensor_tensor(out=b_.t[:, s:, :], in0=a_.t[:, s:, :], in1=a_.t[:, :32 - s, :], op=ALU.add),
                     r=[a_], w=[b_])
                a_, b_ = b_, a_
            pos = k.tile(es, "pos", [128, 32, 8], F32)
            k.op("dve", lambda e: e.tensor_tensor(out=pos.t[:], in0=a_.t[:], in1=cn0.t[:], op=ALU.subtract), r=[a_, cn0], w=[pos])
            k.op("dve", lambda e: e.tensor_tensor(out=pos.t[:].rearrange("p t j -> p (t j)"), in0=pos.t[:].rearrange("p t j -> p (t j)"),
                                                  in1=ppos.t[:], op=ALU.add), r=[pos, ppos], w=[pos])
            val = k.tile(es, "val", [128, 32, 8], F32)
            k.op("dve", lambda e: e.tensor_scalar(out=val.t[:], in0=pos.t[:], scalar1=float(CAP) - 0.5, scalar2=None, op0=ALU.is_lt),
                 r=[pos], w=[val])
            k.op("dve", lambda e: e.tensor_tensor(out=val.t[:], in0=val.t[:], in1=mask.t[:], op=ALU.mult), r=[val, mask], w=[val])
            k.op("dve", lambda e: e.scalar_tensor_tensor(out=pos.t[:], in0=pos.t[:], scalar=-4096.0, in1=val.t[:],
                                                         op0=ALU.add, op1=ALU.mult), r=[pos, val], w=[pos])
            k.op("dve", lambda e: e.tensor_scalar(out=pos.t[:], in0=pos.t[:], scalar1=4096.0, scalar2=None, op0=ALU.add),
                 r=[pos], w=[pos])
            k.op("dve", lambda e: e.tensor_copy(out=pidx.t[:], in_=pos.t[:]), r=[pos], w=[pidx])
            for j in range(EL):
                k.op("dve", lambda e: e.tensor_copy(out=pk.t[:, :, j, 0], in_=gidt.t[:]), r=[gidt], w=[pk])
            k.op("dve", lambda e: e.tensor_copy(out=pk.t[:, :, :, 1], in_=affo.t[:]), r=[affo], w=[pk])
            for t in range(32):
                tok_scatter(0, t)
        k.end_phase()

        with ExitStack() as es:
            XeTs = [k.tile(es, f"XeT{i}", [128, DC, CAP], BF16) for i in range(2)]
            HT = k.tile(es, "HT", [128, NFC, CAP], BF16)
            wq = [k.tile(es, f"wq{i}", [128, DC * 512], BF16) for i in range(4)]
            xe = [k.tile(es, f"xe{i}", [128, D], BF16) for i in range(4)]
            tg = [[k.tile(es, f"tg{p}_{i}", [128, 2], F32) for i in range(4)] for p in range(2)]
            tgi = [[k.tile(es, f"tgi{p}_{i}", [128, 1], I32) for i in range(4)] for p in range(2)]
            yr = [[k.tile(es, f"yr{p}_{i}", [128, 2], F32) for i in range(4)] for p in range(2)]
            yri = [[k.tile(es, f"yri{p}_{i}", [128, 1], I32) for i in range(4)] for p in range(2)]
            yst = [k.tile(es, f"yst{i}", [128, D], F32) for i in range(4)]
            sg = [k.tile(es, f"sg6_{i}", [128, 512], F32) for i in range(2)]
            pb = [k.tile(es, f"pb{i}", [128, 512], F32, psum=True) for i in range(8)]
            tpp = [pb[6], pb[7]]
            tppv = [pb[i].t[:].bitcast(BF16)[:, 0:512].rearrange("p (q t) -> p q t", t=128) for i in (6, 7)]
            cnt6 = {"wn": 0}

            def prep_gather(j):
                p = j % 2
                for st in range(4):
                    k.dma("sp", tg[p][st].t[:], tokg_d[j].t[st * 128:(st + 1) * 128, :], r=[tokg_d[j]], w=[tg[p][st]])
                    x = xe[st]
                    k.op("dve", lambda e: e.tensor_copy(out=tgi[p][st].t[:], in_=tg[p][st].t[:, 0:1]), r=[tg[p][st]], w=[tgi[p][st]])
                    k.idma(out=x.t[:, :], in_=xn2_sh.t[:, :], in_off=tgi[p][st].t[:, 0:1], bounds=4095,
                           r=[xn2_sh, tgi[p][st]], w=[x])
                    k.op("dve", lambda e: e.tensor_tensor(out=yr[p][st].t[:, 1:2], in0=tg[p][st].t[:, 0:1], in1=ybt.t[:], op=ALU.add),
                         r=[tg[p][st], ybt], w=[yr[p][st]])
                    k.op("dve", lambda e: e.tensor_copy(out=yri[p][st].t[:], in_=yr[p][st].t[:, 1:2]), r=[yr[p][st]], w=[yri[p][st]])

            def prep_transpose(j):
                XeT = XeTs[j % 2]
                for st in range(4):
                    x = xe[st]
                    for gq in range(4):
                        tp = tpp[gq % 2]
                        tv_ = tppv[gq % 2]
                        for q in range(4):
                            dc = gq * 4 + q
                            k.op("pe", lambda e: e.transpose(tv_[:, q, :], x.t[:, dc * 128:(dc + 1) * 128], identb),
                                 r=[x, cbf], w=[tp])
                        if gq % 2 == 0:
                            k.op("act", lambda e: e.copy(out=XeT.t[:, gq * 4:(gq + 1) * 4, st * 128:(st + 1) * 128], in_=tv_),
                                 r=[tp], w=[XeT])
                        else:
                            k.op("dve", lambda e: e.tensor_copy(out=XeT.t[:, gq * 4:(gq + 1) * 4, st * 128:(st + 1) * 128], in_=tv_),
                                 r=[tp], w=[XeT])

            prep_gather(0)
            prep_transpose(0)
            gathered = {0}
            deferred = []
            for j in range(EL):
                p = j % 2
                XeT = XeTs[p]
                pend = [(j + 1, t) for t in range(32)] if j + 1 < EL else []
                pre = j < NPRE
                wq_eng = "sp" if pre else "pool"
                wsrc = [wgb, wub, wdb] if pre else []
                wgv = (wgb if pre else wg).t[j].rearrange("(dc p) f -> p dc f", p=128)
                wuv = (wub if pre else wu).t[j].rearrange("(dc p) f -> p dc f", p=128)
                for gi, f0 in enumerate(range(0, FF, 512)):
                    nf = min(512, FF - f0)
                    wn = cnt6["wn"]
                    tG = wq[wn % 4]
                    tU = wq[(wn + 1) % 4]
                    k.dma(wq_eng, tG.t[:, 0:DC * nf].rearrange("p (dc f) -> p dc f", f=nf), wgv[:, :, f0:f0 + nf], r=wsrc, w=[tG])
                    k.dma(wq_eng, tU.t[:, 0:DC * nf].rearrange("p (dc f) -> p dc f", f=nf), wuv[:, :, f0:f0 + nf], r=wsrc, w=[tU])
                    cnt6["wn"] += 2
                    if gi == 1:
                        while deferred:
                            deferred.pop(0)()
                    for _ in range(8):
                        if pend:
                            tok_scatter(*pend.pop(0))
                    if j + 1 < EL and not pend and (j + 1) not in gathered:
                        gathered.add(j + 1)
                        prep_gather(j + 1)
                    tGv = tG.t[:, 0:DC * nf].rearrange("p (dc f) -> p dc f", f=nf)
                    tUv = tU.t[:, 0:DC * nf].rearrange("p (dc f) -> p dc f", f=nf)
                    for fcl in range(nf // 128):
                        fc = f0 // 128 + fcl
                        pG = pb[(2 * fc) % 6]
                        pU = pb[(2 * fc + 1) % 6]
                        for (pp, tv, tt_) in ((pG, tGv, tG), (pU, tUv, tU)):
                            for dc in range(DC):
                                k.op("pe", lambda e: e.matmul(pp.t[:], lhsT=tv[:, dc, fcl * 128:(fcl + 1) * 128], rhs=XeT.t[:, dc, :],
                                                              start=(dc == 0), stop=(dc == DC - 1)), r=[tt_, XeT], w=[pp])
                        s_ = sg[fc % 2]
                        k.op("act", lambda e: e.activation(out=s_.t[:], in_=pG.t[:], func=AF.Silu), r=[pG], w=[s_])
                        k.op("dve", lambda e: e.tensor_tensor(out=HT.t[:, fc, :], in0=pU.t[:], in1=s_.t[:], op=ALU.mult),
                             r=[pU, s_], w=[HT])
                while deferred:
                    deferred.pop(0)()
                while pend:
                    tok_scatter(*pend.pop(0))
                if j + 1 < EL:
                    if (j + 1) not in gathered:
                        gathered.add(j + 1)
                        prep_gather(j + 1)
                    prep_transpose(j + 1)
                wdv = (wdb if pre else wd).t[j].rearrange("(fc p) n -> p fc n", p=128)
                for half in range(2):
                    c0 = half * 1024
                    for g0 in range(0, NFC, 8):
                        ng = min(8, NFC - g0)
                        tW = wq[cnt6["wn"] % 4]
                        k.dma(wq_eng, tW.t[:, 0:ng * 1024].rearrange("p (fc n) -> p fc n", n=1024), wdv[:, g0:g0 + ng, c0:c0 + 1024],
                              r=wsrc, w=[tW])
                        cnt6["wn"] += 1
                        tWv = tW.t[:, 0:ng * 1024].rearrange("p (fc n) -> p fc n", n=1024)
                        for fl in range(ng):
                            fc = g0 + fl
                            for st in range(4):
                                for nb in range(2):
                                    pp = pb[st * 2 + nb]
                                    k.op("pe", lambda e: e.matmul(pp.t[:], lhsT=HT.t[:, fc, st * 128:(st + 1) * 128],
                                                                  rhs=tWv[:, fl, nb * 512:(nb + 1) * 512], start=(fc == 0), stop=(fc == NFC - 1)),
                                         r=[HT, tW], w=[pp])
                    for st in range(4):
                        gate = tg[p][st].t[:, 1:2]
                        for nb in range(2):
                            pp = pb[st * 2 + nb]
                            osl = yst[st].t[:, c0 + nb * 512: c0 + (nb + 1) * 512]
                            if nb == 0:
                                k.op("act", lambda e: e.activation(out=osl, in_=pp.t[:], func=AF.Copy, scale=gate), r=[pp, tg[p][st]], w=[yst[st]])
                            else:
                                k.op("dve", lambda e: e.tensor_scalar(out=osl, in0=pp.t[:], scalar1=gate, scalar2=None, op0=ALU.mult),
                                     r=[pp, tg[p][st]], w=[yst[st]])
                def emit_scatter_add(p=p):
                    for st in range(4):
                        k.idma(out=yacc_sh.t[:, :], in_=yst[st].t[:, :], out_off=yri[p][st].t[:, 0:1], bounds=8191,
                               r=[yst[st], yri[p][st]], w=[yacc_sh], cop=ALU.add)
                if j + 1 < EL:
                    deferred.append(emit_scatter_add)
                else:
                    emit_scatter_add()
        k.pair_barrier(bsrc, bdst)

        with ExitStack() as es:
            g2b = k.tile(es, "g2b", [128, D], F32)
            fw = k.tile(es, "fw", [128, D], F32)
            load_bc(g2b, ada_d, 5 * D)
            load_bc(fw, fnw, 0)
            x1t = [k.tile(es, f"x7_{i}", [128, D], F32) for i in range(2)]
            ya = [k.tile(es, f"ya{i}", [128, D], F32) for i in range(2)]
            yb = [k.tile(es, f"yb{i}", [128, D], F32) for i in range(2)]
            ot = [k.tile(es, f"ot{i}", [128, D], F32) for i in range(2)]
            junk = k.tile(es, "junk7", [128, D], BF16)
            ss = k.tile(es, "ss7", [128, 2], F32)
            for tt in range(16):
                i = tt % 2
                k.dma("sp", x1t[i].t[:], x1_d.t[tt * 128:(tt + 1) * 128, :], r=[x1_d], w=[x1t[i]])
                k.idma(out=ya[i].t[:, :], in_=yacc_sh.t[:, :], in_off=idx_ya.t[:, tt:tt + 1], bounds=8191,
                       r=[yacc_sh, idx_ya], w=[ya[i]])
                k.idma(out=yb[i].t[:, :], in_=yacc_sh.t[:, :], in_off=idx_yb.t[:, tt:tt + 1], bounds=8191,
                       r=[yacc_sh, idx_yb], w=[yb[i]])
                k.op("dve", lambda e: e.tensor_tensor(out=ya[i].t[:], in0=ya[i].t[:], in1=yb[i].t[:], op=ALU.add),
                     r=[ya[i], yb[i]], w=[ya[i]])
                k.op("dve", lambda e: e.tensor_tensor(out=ya[i].t[:], in0=ya[i].t[:], in1=g2b.t[:], op=ALU.mult),
                     r=[ya[i], g2b], w=[ya[i]])
                k.op("dve", lambda e: e.tensor_tensor(out=ya[i].t[:], in0=ya[i].t[:], in1=x1t[i].t[:], op=ALU.add),
                     r=[ya[i], x1t[i]], w=[ya[i]])
                k.op("act", lambda e: e.activation(out=junk.t[:], in_=ya[i].t[:], func=AF.Square, accum_out=ss.t[:, 0:1]),
                     r=[ya[i]], w=[junk, ss])
                k.op("act", lambda e: e.activation(out=ss.t[:, 1:2], in_=ss.t[:, 0:1], func=AF.Sqrt, scale=1.0 / D, bias=epsb.t[:, 0:1]),
                     r=[ss, epsb], w=[ss])
                k.op("dve", lambda e: e.reciprocal(out=ss.t[:, 1:2], in_=ss.t[:, 1:2]), r=[ss], w=[ss])
                k.op("dve", lambda e: e.scalar_tensor_tensor(out=ot[i].t[:], in0=ya[i].t[:], scalar=ss.t[:, 1:2], in1=fw.t[:],
                                                             op0=ALU.mult, op1=ALU.mult), r=[ya[i], ss, fw], w=[ot[i]])
                k.dma("act", out.t[tt * 128:(tt + 1) * 128, :], ot[i].t[:], r=[ot[i]], w=[out])
        k.sync_all()
    return nc


def make_consts():
    i = np.arange(128)
    ident = np.eye(128, dtype=np.float32)
    triF = (i[:, None] <= i[None, :]).astype(np.float32)
    triB = (i[:, None] >= i[None, :]).astype(np.float32)
    ones = np.ones((128, 128), np.float32)
    strictL = (i[:, None] < i[None, :]).astype(np.float32)
    return np.ascontiguousarray(np.concatenate([ident, triF, triB, ones, strictL], axis=1))


def shard_inputs(x, c, ctx, c_ctx, w_ada, b_ada, norm1_w, w_in, mlstm_b_i, mlstm_b_f, mlstm_norm_w,
                 conv_w, conv_b, conv_norm_w, conv_norm_b, w_out, norm2_w, w_router, w_gate, w_up,
                 w_down, final_norm_w):
    f = np.float32
    consts = make_consts()
    p = np.arange(128)
    in_maps = []
    w_ada0 = np.ascontiguousarray(w_ada[0], f)
    w_out0 = np.ascontiguousarray(w_out[0], f)
    w_in0 = np.asarray(w_in[0], f)
    w_in_sw = w_in0.copy()
    w_in_sw[:, 4096:4104] = w_in0[:, 4104:4112]
    w_in_sw[:, 4104:4112] = w_in0[:, 4096:4104]
    wr_arr = np.ascontiguousarray(np.asarray(w_router[0], f).reshape(16, 128, 16).transpose(1, 0, 2))
    for core in range(8):
        b, r = core // 2, core % 2
        tau = np.arange(4096)
        posn = tau if r == 0 else 4095 - tau
        xall = np.ascontiguousarray(np.asarray(x[b], f)[posn])
        ctxl = np.asarray(ctx[b], f)
        ctxl = np.ascontiguousarray(ctxl if r == 0 else ctxl[::-1])
        cs2 = np.stack([np.asarray(c[b], f), np.asarray(c_ctx, f)], axis=-1)
        cs = np.ascontiguousarray(cs2.reshape(16, 128, 2).transpose(1, 0, 2))
        bi, bf_ = np.asarray(mlstm_b_i[0], f), np.asarray(mlstm_b_f[0], f)
        if r == 0:
            gbias = np.concatenate([bi[0], bf_[0], bi[1], bf_[1]])
        else:
            gbias = np.concatenate([bi[1], bf_[1], bi[0], bf_[0]])
        cwk = np.asarray(conv_w[0], f)
        if r == 1:
            cwk = cwk[::-1]
        convw = np.ascontiguousarray(cwk.T.reshape(8, 128, KT).transpose(1, 0, 2))

        def col8(v):
            return np.ascontiguousarray(np.asarray(v, f).reshape(8, 128).T)
        own_pos = posn[:NOWN]
        own_gid = np.ascontiguousarray(own_pos.reshape(16, 128).T.astype(np.int32))
        tp = (np.arange(32)[None, :] * 128 + p[:, None]).astype(np.int32)
        m = {
            "xall": xall, "ctxl": ctxl, "cs": cs,
            "w_ada": w_ada0, "b_ada": np.asarray(b_ada[0], f).reshape(1, -1),
            "n1w": np.asarray(norm1_w[0], f).reshape(1, -1), "n2w": np.asarray(norm2_w[0], f).reshape(1, -1),
            "fnw": np.asarray(final_norm_w, f).reshape(1, -1),
            "w_in": w_in0 if r == 0 else w_in_sw,
            "gbias": gbias.reshape(1, 16).astype(f),
            "mnw": np.asarray(mlstm_norm_w[0], f).reshape(1, -1),
            "convw": convw, "convb": col8(conv_b[0]), "cnw": col8(conv_norm_w[0]), "cnb": col8(conv_norm_b[0]),
            "w_out": w_out0, "w_r": wr_arr,
            "wg": np.asarray(w_gate[0, r * 8:(r + 1) * 8], f), "wu": np.asarray(w_up[0, r * 8:(r + 1) * 8], f),
            "wd": np.asarray(w_down[0, r * 8:(r + 1) * 8], f),
            "consts": consts,
            "own_gid": own_gid, "own_gid2": (own_gid + 4096).astype(np.int32),
            "zrows": (tp + r * 4096).astype(np.int32),
            "yrowsA": own_gid, "yrowsB": (own_gid + 4096).astype(np.int32),
            "affrows": (tp + r * 4096).astype(np.int32),
            "gid_f": tp.astype(np.float32),
            "ybase": np.full((128, 1), float(r * 4096), f),
        }
        in_maps.append(m)
    return in_maps


_NC_CACHE = {}


def kernel(**inputs):
    FF = int(np.asarray(inputs["w_gate"]).shape[-1])
    if FF not in _NC_CACHE:
        _NC_CACHE[FF] = build(FF)
    nc = _NC_CACHE[FF]
    in_maps = shard_inputs(**inputs)
    res = run_bass_kernel_spmd(nc, in_maps, core_ids=list(range(8)))
    B = np.asarray(inputs["x"]).shape[0]
    outp = np.zeros((B, 4096, D), np.float32)
    for core in range(8):
        b, r = core // 2, core % 2
        o = np.asarray(res.results[core]["out"], np.float32)
        if r == 0:
            outp[b, 0:NOWN] = o
        else:
            outp[b, 4095 - np.arange(NOWN)] = o
    return outp
```

```python
import numpy as np
from contextlib import ExitStack
import concourse.bass as bass
import concourse.mybir as mybir
from concourse.bass_utils import run_bass_kernel_spmd

F32 = mybir.dt.float32
BF16 = mybir.dt.bfloat16
I32 = mybir.dt.int32
AF = mybir.ActivationFunctionType
ALU = mybir.AluOpType
AX = mybir.AxisListType

D = 2048
DC = 16
NH = 4
DH = 256
W = 1024
CW = 1024
KT = 31
EL = 8
CAP = 512
NOWN = 2048
NCTX = 256
NCH = 34
EPS = 1e-6
INC = 6160
NBIS = 28


class Res:
    __slots__ = ("w", "r")

    def __init__(self):
        self.w = None
        self.r = {}


class Tile:
    def __init__(self, t, name="", onchip=False):
        self.t = t
        self.res = Res()
        self.name = name
        self.onchip = onchip


class Eng:
    def __init__(self, name, e, sem):
        self.name = name
        self.e = e
        self.sem = sem
        self.n = 0
        self.waited = {}


class DS:
    def __init__(self, sem):
        self.sem = sem
        self.n = 0


class KB:
    def __init__(self, nc):
        self.nc = nc
        self.E = {}
        for name, e in (("pe", nc.tensor), ("act", nc.scalar), ("dve", nc.vector),
                        ("pool", nc.gpsimd), ("sp", nc.sync)):
            self.E[name] = Eng(name, e, nc.alloc_semaphore(name="sem_" + name))
        self.ds = {}
        self.free_ds = []
        self.keep = {"misc", "yz"}
        self.nosync = set()
        self.tiles = []
        self.ccsem = nc.alloc_semaphore(name="sem_cc")
        self.ccn = 0
        self._bregs = {}

    def tile(self, es, name, shape, dt, psum=False):
        if psum:
            t = es.enter_context(self.nc.psum_tensor(name, list(shape), dt))
        else:
            t = es.enter_context(self.nc.sbuf_tensor(name, list(shape), dt))
        T = Tile(t, name, True)
        self.tiles.append(T)
        return T

    def dram(self, name, shape, dt, **kw):
        T = Tile(self.nc.dram_tensor(name, list(shape), dt, **kw), name)
        self.tiles.append(T)
        return T

    def _waits(self, en, r, w):
        E = self.E[en]
        need = {}

        def add(ev):
            if ev is None:
                return
            sem, val, owner = ev
            if owner == "pe" and en == "pe":
                return
            k = id(sem)
            if k not in need or need[k][1] < val:
                need[k] = (sem, val)
        for x in r:
            add(x.res.w)
        for x in w:
            add(x.res.w)
            for ev in x.res.r.values():
                add(ev)
        for sem, val in need.values():
            if E.waited.get(id(sem), 0) < val:
                E.e.wait_ge(sem, val)
                E.waited[id(sem)] = val

    def _post(self, ev, r, w):
        for x in r:
            x.res.r[id(ev[0])] = ev
        for x in w:
            x.res.w = ev
            x.res.r = {}

    def op(self, en, fn, r=(), w=()):
        E = self.E[en]
        self._waits(en, r, w)
        ins = fn(E.e)
        E.n += 1
        ins.then_inc(E.sem, 1)
        self._post((E.sem, E.n, en), r, w)

    def dsem(self, key):
        if key not in self.ds:
            if self.free_ds:
                self.ds[key] = self.free_ds.pop()
            else:
                self.ds[key] = DS(self.nc.alloc_semaphore(name="dsem%d" % len(self.ds)))
        return self.ds[key]

    def breg(self, val):
        if val not in self._bregs:
            self._bregs[val] = self.nc.gpsimd.to_reg(val)
        return self._bregs[val]

    def _key(self, sem, r, w):
        if sem is not None:
            return sem
        for x in w:
            if x.onchip:
                return x.name
        for x in r:
            if x.onchip:
                return x.name
        return "misc"

    def end_phase(self):
        self.sync_all()
        for key in list(self.ds.keys()):
            if key not in self.keep:
                self.free_ds.append(self.ds.pop(key))

    def dma(self, en, out, in_, r=(), w=(), sem=None, **kw):
        E = self.E[en]
        self._waits(en, r, w)
        ins = E.e.dma_start(out=out, in_=in_, **kw)
        d = self.dsem(self._key(sem, r, w))
        d.n += 16
        ins.then_inc(d.sem, 16)
        self._post((d.sem, d.n, None), r, w)

    def idma(self, out, in_, out_off=None, in_off=None, bounds=0, r=(), w=(), sem=None, cop=None):
        E = self.E["pool"]
        self._waits("pool", r, w)
        kw = {}
        if cop is not None:
            kw["compute_op"] = cop
        ins = E.e.indirect_dma_start(
            out=out,
            out_offset=None if out_off is None else bass.IndirectOffsetOnAxis(ap=out_off, axis=0),
            in_=in_,
            in_offset=None if in_off is None else bass.IndirectOffsetOnAxis(ap=in_off, axis=0),
            bounds_check=self.breg(bounds), oob_is_err=False, **kw)
        d = self.dsem(self._key(sem, r, w))
        d.n += 16
        ins.then_inc(d.sem, 16)
        self._post((d.sem, d.n, None), r, w)

    def sync_all(self):
        for E in self.E.values():
            for Fn in self.E.values():
                if Fn is not E and Fn.n > 0 and E.waited.get(id(Fn.sem), 0) < Fn.n:
                    E.e.wait_ge(Fn.sem, Fn.n)
                    E.waited[id(Fn.sem)] = Fn.n
            for key, d in self.ds.items():
                if key in self.nosync:
                    continue
                if d.n > 0 and E.waited.get(id(d.sem), 0) < d.n:
                    E.e.wait_ge(d.sem, d.n)
                    E.waited[id(d.sem)] = d.n
        for T in self.tiles:
            if getattr(T, "persist", False):
                continue
            T.res.w = None
            T.res.r = {}

    def pair_barrier(self, src, dst):
        self.sync_all()
        self.E["pool"].e.collective_compute(
            "AllReduce", ALU.add, replica_groups=[[0, 1], [2, 3], [4, 5], [6, 7]],
            ins=[src.t.ap().opt()], outs=[dst.t.ap().opt()]).then_inc(self.ccsem)
        self.ccn += 1
        for E in self.E.values():
            E.e.wait_ge(self.ccsem, self.ccn)


def bcast_rows(h, off, n, parts=128):
    a = h.ap()
    return bass.AP(tensor=a.tensor, offset=off, ap=[[0, parts], [1, n]])


def build(FF=5504, dbg=False):
    nc = bass.Bass("TRN2", target_bir_lowering=False)
    k = KB(nc)
    NFC = FF // 128

    def din(name, shape, dt=F32):
        return Tile(nc.dram_tensor(name, list(shape), dt, kind="ExternalInput"), name)

    xall = din("xall", [4096, D])
    ctxl = din("ctxl", [NCTX, D])
    cs = din("cs", [128, DC, 2])
    w_ada = din("w_ada", [D, 6 * D])
    b_ada = din("b_ada", [1, 6 * D])
    n1w = din("n1w", [1, D])
    n2w = din("n2w", [1, D])
    fnw = din("fnw", [1, D])
    w_in = din("w_in", [D, INC])
    gbias = din("gbias", [1, 16])
    mnw = din("mnw", [1, W])
    convw = din("convw", [128, 8, KT])
    convb = din("convb", [128, 8])
    cnw = din("cnw", [128, 8])
    cnb = din("cnb", [128, 8])
    w_out = din("w_out", [D, D])
    w_r = din("w_r", [128, DC, 16])
    wg = din("wg", [EL, D, FF])
    wu = din("wu", [EL, D, FF])
    wd = din("wd", [EL, FF, D])
    consts = din("consts", [128, 5 * 128])
    own_gid = din("own_gid", [128, 16], I32)
    own_gid2 = din("own_gid2", [128, 16], I32)
    zrows = din("zrows", [128, 32], I32)
    yrowsA = din("yrowsA", [128, 16], I32)
    yrowsB = din("yrowsB", [128, 16], I32)
    affrows = din("affrows", [128, 32], I32)
    gid_f = din("gid_f", [128, 32])
    ybase = din("ybase", [128, 1])
    out = Tile(nc.dram_tensor("out", [NOWN, D], F32, kind="ExternalOutput"), "out")
    k.tiles += [xall, ctxl, out]

    ada_d = k.dram("ada_d", [2, 6 * D], F32)
    qT_d = k.dram("qT_d", [16, 128, 1024], BF16)
    kT_d = k.dram("kT_d", [NCH, 128, 1024], BF16)
    k_d = k.dram("k_d", [NCH * 128, W], BF16)
    v_d = k.dram("v_d", [NCH * 128, W], BF16)
    o_d = k.dram("o_d", [NOWN, W], F32)
    g_d = k.dram("g_d", [NCH * 128, 16], F32)
    uT_d = k.dram("uT_d", [8, 128, NOWN], BF16)
    h1_d = k.dram("h1_d", [NOWN, W], F32)
    mixT_d = k.dram("mixT_d", [16, 128, NOWN], BF16)
    x1_d = k.dram("x1_d", [NOWN, D], F32)
    xn2_sh = k.dram("xn2_sh", [4096, D], BF16, addr_space="Shared")
    aff_sh = k.dram("aff_sh", [8192, 16], F32, addr_space="Shared")
    yacc_sh = k.dram("yacc_sh", [8192, D], F32, addr_space="Shared")
    tokg_d = [k.dram(f"tokg_d{j}", [CAP, 2], F32) for j in range(EL)]
    NPRE = 0
    RUN = 1376 if FF % 1376 == 0 else FF
    wgb = wub = wdb = None
    if NPRE:
        wgb = k.dram("wgb", [NPRE, D, FF], BF16)
        wub = k.dram("wub", [NPRE, D, FF], BF16)
        wdb = k.dram("wdb", [NPRE, FF, D], BF16)
    bsrc = k.dram("bsrc", [128, 128], F32)
    bdst = k.dram("bdst", [128, 128], F32)
    if dbg:
        dbg_x1 = Tile(nc.dram_tensor("dbg_x1", [NOWN, D], F32, kind="ExternalOutput"))
        dbg_mix = Tile(nc.dram_tensor("dbg_mix", [16, 128, NOWN], BF16, kind="ExternalOutput"))
        dbg_aff = Tile(nc.dram_tensor("dbg_aff", [8192, 16], F32, kind="ExternalOutput"))

    with ExitStack() as g:
        cst = k.tile(g, "cst", [128, 5 * 128], F32)
        ident = cst.t[:, 0:128]
        triF = cst.t[:, 128:256]
        triB = cst.t[:, 256:384]
        ones = cst.t[:, 384:512]
        strictL = cst.t[:, 512:640]
        cbf = k.tile(g, "cbf", [128, 3 * 128], BF16)
        identb = cbf.t[:, 0:128]
        maskFb = cbf.t[:, 128:256]
        maskBb = cbf.t[:, 256:384]
        idx_own = k.tile(g, "idx_own", [128, 16], I32)
        idx_own2 = k.tile(g, "idx_own2", [128, 16], I32)
        idx_z = k.tile(g, "idx_z", [128, 32], I32)
        idx_ya = k.tile(g, "idx_ya", [128, 16], I32)
        idx_yb = k.tile(g, "idx_yb", [128, 16], I32)
        idx_aff = k.tile(g, "idx_aff", [128, 32], I32)
        gidt = k.tile(g, "gidt", [128, 32], F32)
        ybt = k.tile(g, "ybt", [128, 1], F32)
        epsb = k.tile(g, "epsb", [128, 1], F32)
        pidx = k.tile(g, "pidx", [128, 32, 8], I32)
        pk = k.tile(g, "pk", [128, 32, 8, 2], F32)
        es_zt = ExitStack()
        zt = k.tile(es_zt, "zt", [128, D], F32)

        k.dma("sp", cst.t[:], consts.t.ap(), w=[cst])
        for tl, src in ((idx_own, own_gid), (idx_own2, own_gid2), (idx_z, zrows), (idx_ya, yrowsA),
                        (idx_yb, yrowsB), (idx_aff, affrows), (gidt, gid_f), (ybt, ybase)):
            k.dma("sp", tl.t[:], src.t.ap(), w=[tl])
        k.op("dve", lambda e: e.tensor_copy(out=cbf.t[:, 0:128], in_=cst.t[:, 0:128]), r=[cst], w=[cbf])
        k.op("dve", lambda e: e.tensor_copy(out=cbf.t[:, 128:384], in_=cst.t[:, 128:384]), r=[cst], w=[cbf])
        k.op("pool", lambda e: e.memset(zt.t[:], 0.0), w=[zt])
        k.op("pool", lambda e: e.memset(epsb.t[:], EPS), w=[epsb])
        k.dma("pool", bsrc.t.ap(), zt.t[:, 0:128], r=[zt], w=[bsrc])
        for t in range(32):
            k.idma(out=yacc_sh.t[:, :], in_=zt.t[:, :], out_off=idx_z.t[:, t:t + 1], bounds=8191,
                   r=[zt, idx_z], w=[yacc_sh], sem="yz")

        with ExitStack() as es:
            cs_t = k.tile(es, "cs_t", [128, DC, 2], F32)
            cs_s = k.tile(es, "cs_s", [128, DC, 2], F32)
            wt = [k.tile(es, f"wada{i}", [128, DC, 512], F32) for i in range(2)]
            bt = k.tile(es, "bada", [2, 6 * D], F32)
            ada_sb = k.tile(es, "ada_sb", [2, 6 * D], F32)
            ps = [k.tile(es, f"adaps{i}", [128, 512], F32, psum=True) for i in range(2)]
            k.dma("sp", cs_t.t[:], cs.t.ap(), w=[cs_t])
            k.dma("sp", bt.t[:], bcast_rows(b_ada.t, 0, 6 * D, parts=2), w=[bt])
            k.op("act", lambda e: e.activation(out=cs_s.t[:], in_=cs_t.t[:], func=AF.Silu), r=[cs_t], w=[cs_s])
            wav = w_ada.t.ap().rearrange("(kc p) n -> p kc n", p=128)
            for cb in range(24):
                wtt = wt[cb % 2]
                pst = ps[cb % 2]
                k.dma("sp", wtt.t[:], wav[:, :, cb * 512:(cb + 1) * 512], w=[wtt])
                for kc in range(DC):
                    k.op("pe", lambda e: e.matmul(pst.t[0:2, :], lhsT=cs_s.t[:, kc, :], rhs=wtt.t[:, kc, :],
                                                  start=(kc == 0), stop=(kc == DC - 1)),
                         r=[cs_s, wtt], w=[pst])
                k.op("dve", lambda e: e.tensor_tensor(out=ada_sb.t[:, cb * 512:(cb + 1) * 512], in0=pst.t[0:2, :],
                                                      in1=bt.t[:, cb * 512:(cb + 1) * 512], op=ALU.add),
                     r=[pst, bt], w=[ada_sb])
            k.dma("sp", ada_d.t.ap(), ada_sb.t[:], r=[ada_sb], w=[ada_d])
        k.end_phase()
        es_zt.close()

        def load_bc(tl, src, off, n=D, eng="sp"):
            k.dma(eng, tl.t[:, 0:n], bcast_rows(src.t, off, n), w=[tl])

        with ExitStack() as es:
            A_t = k.tile(es, "A_t", [128, D], F32)
            B_t = k.tile(es, "B_t", [128, D], F32)
            nw = k.tile(es, "nw", [128, D], F32)
            gb = k.tile(es, "gb", [128, 16], F32)
            load_bc(nw, n1w, 0)
            load_bc(gb, gbias, 0, 16)

            def load_AB(row):
                load_bc(B_t, ada_d, row * 6 * D)
                load_bc(A_t, ada_d, row * 6 * D + D)
                k.op("dve", lambda e: e.scalar_tensor_tensor(out=A_t.t[:], in0=A_t.t[:], scalar=1.0, in1=nw.t[:],
                                                             op0=ALU.add, op1=ALU.mult), r=[A_t, nw], w=[A_t])
            xnT = k.tile(es, "xnT", [128, DC, NOWN], BF16)
            xt = [k.tile(es, f"xt{i}", [128, D], F32) for i in range(2)]
            tmpf = k.tile(es, "tmpf", [128, D], F32)
            junk = tmpf
            xnb = [k.tile(es, "xnb0", [128, D], BF16)]
            ss = k.tile(es, "ss", [128, 2], F32)
            tps = [k.tile(es, f"tps{i}", [128, 4, 128], BF16, psum=True) for i in range(2)]
            wts = [k.tile(es, f"wt{i}", [128, DC, 512], BF16) for i in range(2)]
            acc = [k.tile(es, f"acc{i}", [128, 512], F32, psum=True) for i in range(4)]
            stb = [k.tile(es, f"stb{i}", [128, 512], BF16) for i in range(3)]
            stf = [k.tile(es, f"stf{i}", [128, 512], F32) for i in range(3)]
            sig = [k.tile(es, f"sig{i}", [128, 512], F32) for i in range(2)]
            cnt = {"w": 0, "a": 0, "sb": 0, "sf": 0, "sg": 0, "ev": 0}
            wv = w_in.t.ap().rearrange("(dc p) n -> p dc n", p=128)

            def load_w(c0, n):
                t = wts[cnt["w"] % 2]
                cnt["w"] += 1
                k.dma("pool", t.t[:, :, 0:n], wv[:, :, c0:c0 + n], w=[t])
                return t

            def nxt(lst, key):
                t = lst[cnt[key] % len(lst)]
                cnt[key] += 1
                return t

            def build_xnT(src, row0, ntiles, A, B):
                for tt in range(ntiles):
                    x = xt[tt % 2]
                    k.dma("sp", x.t[:], src.t[row0 + tt * 128: row0 + (tt + 1) * 128, :], w=[x])
                    k.op("act", lambda e: e.activation(out=junk.t[:], in_=x.t[:], func=AF.Square,
                                                       accum_out=ss.t[:, 0:1]), r=[x], w=[junk, ss])
                    k.op("act", lambda e: e.activation(out=ss.t[:, 1:2], in_=ss.t[:, 0:1], func=AF.Sqrt, scale=1.0 / D, bias=epsb.t[:, 0:1]),
                         r=[ss, epsb], w=[ss])
                    k.op("dve", lambda e: e.reciprocal(out=ss.t[:, 1:2], in_=ss.t[:, 1:2]), r=[ss], w=[ss])
                    k.op("dve", lambda e: e.scalar_tensor_tensor(out=tmpf.t[:], in0=x.t[:], scalar=ss.t[:, 1:2],
                                                                 in1=A.t[:], op0=ALU.mult, op1=ALU.mult),
                         r=[x, ss, A], w=[tmpf])
                    xb = xnb[0]
                    k.op("dve", lambda e: e.tensor_tensor(out=xb.t[:], in0=tmpf.t[:], in1=B.t[:], op=ALU.add),
                         r=[tmpf, B], w=[xb])
                    for gq in range(4):
                        tp = tps[gq % 2]
                        for q in range(4):
                            dc = gq * 4 + q
                            k.op("pe", lambda e: e.transpose(tp.t[:, q, :], xb.t[:, dc * 128:(dc + 1) * 128], identb),
                                 r=[xb, cbf], w=[tp])
                        en = "act" if gq % 2 == 0 else "dve"
                        if en == "act":
                            k.op("act", lambda e: e.copy(out=xnT.t[:, gq * 4:(gq + 1) * 4, tt * 128:(tt + 1) * 128],
                                                         in_=tp.t[:]), r=[tp], w=[xnT])
                        else:
                            k.op("dve", lambda e: e.tensor_copy(out=xnT.t[:, gq * 4:(gq + 1) * 4, tt * 128:(tt + 1) * 128],
                                                                in_=tp.t[:]), r=[tp], w=[xnT])

            def tm_cols(wtile, ncols, ntiles, evac):
                for tt in range(ntiles):
                    a = nxt(acc, "a")
                    for dc in range(DC):
                        k.op("pe", lambda e: e.matmul(a.t[:, 0:ncols], lhsT=xnT.t[:, dc, tt * 128:(tt + 1) * 128],
                                                      rhs=wtile.t[:, dc, 0:ncols], start=(dc == 0), stop=(dc == DC - 1)),
                             r=[xnT, wtile], w=[a])
                    evac(a, tt)

            def fm_cols(wtile, ntok, evac, ncc=4):
                tb = min(512, ntok)
                for cc in range(ncc):
                    for b0 in range(0, ntok, tb):
                        a = nxt(acc, "a")
                        for dc in range(DC):
                            k.op("pe", lambda e: e.matmul(a.t[:, 0:tb], lhsT=wtile.t[:, dc, cc * 128:(cc + 1) * 128],
                                                          rhs=xnT.t[:, dc, b0:b0 + tb], start=(dc == 0), stop=(dc == DC - 1)),
                                 r=[xnT, wtile], w=[a])
                        evac(a, cc, b0, tb)

            def evac_engine():
                cnt["ev"] += 1
                return "act" if cnt["ev"] % 2 == 0 else "dve"

            def scaled_copy(en, o, i, scale, r, w):
                if en == "act":
                    k.op("act", lambda e: e.activation(out=o, in_=i, func=AF.Copy, scale=scale), r=r, w=w)
                else:
                    k.op("dve", lambda e: e.tensor_scalar(out=o, in0=i, scalar1=scale, scalar2=None, op0=ALU.mult),
                         r=r, w=w)

            def project(ntok, chunk0, full):
                ntiles = ntok // 128
                nchk = ntiles
                for half in range(2):
                    wtile = load_w(1024 + half * 512, 512)

                    def ev_kT(a, cc, b0, tb, half=half):
                        s = nxt(stb, "sb")
                        scaled_copy(evac_engine(), s.t[:, 0:tb], a.t[:, 0:tb], 1.0 / 16.0, [a], [s])
                        c0 = chunk0 + b0 // 128
                        hd = half * 4 + cc
                        k.dma("act", kT_d.t[c0:c0 + tb // 128, :, hd * 128:(hd + 1) * 128].rearrange("c p t -> p c t"),
                              s.t[:, 0:tb].rearrange("p (c t) -> p c t", t=128), r=[s], w=[kT_d])
                    fm_cols(wtile, ntok, ev_kT)

                    def ev_k(a, tt, half=half):
                        s = nxt(stb, "sb")
                        scaled_copy(evac_engine(), s.t[:], a.t[:], 1.0 / 16.0, [a], [s])
                        k.dma("act", k_d.t[(chunk0 + tt) * 128:(chunk0 + tt + 1) * 128, half * 512:(half + 1) * 512],
                              s.t[:], r=[s], w=[k_d])
                    tm_cols(wtile, 512, ntiles, ev_k)
                for half in range(2):
                    wtile = load_w(2048 + half * 512, 512)

                    def ev_v(a, tt, half=half):
                        s = nxt(stb, "sb")
                        scaled_copy(evac_engine(), s.t[:], a.t[:], 1.0, [a], [s])
                        k.dma("act", v_d.t[(chunk0 + tt) * 128:(chunk0 + tt + 1) * 128, half * 512:(half + 1) * 512],
                              s.t[:], r=[s], w=[v_d])
                    tm_cols(wtile, 512, ntiles, ev_v)
                wtile = load_w(4096, 16)

                def ev_g(a, tt):
                    s = nxt(stf, "sf")
                    k.op("dve", lambda e: e.tensor_tensor(out=s.t[:, 0:16], in0=a.t[:, 0:16], in1=gb.t[:], op=ALU.add),
                         r=[a, gb], w=[s])
                    k.dma("act", g_d.t[(chunk0 + tt) * 128:(chunk0 + tt + 1) * 128, :], s.t[:, 0:16], r=[s], w=[g_d])
                tm_cols(wtile, 16, ntiles, ev_g)
                if not full:
                    return
                for half in range(2):
                    wtile = load_w(half * 512, 512)

                    def ev_q(a, cc, b0, tb, half=half):
                        s = nxt(stb, "sb")
                        scaled_copy(evac_engine(), s.t[:, 0:tb], a.t[:, 0:tb], 1.0, [a], [s])
                        c0 = b0 // 128
                        hd = half * 4 + cc
                        k.dma("act", qT_d.t[c0:c0 + tb // 128, :, hd * 128:(hd + 1) * 128].rearrange("c p t -> p c t"),
                              s.t[:, 0:tb].rearrange("p (c t) -> p c t", t=128), r=[s], w=[qT_d])
                    fm_cols(wtile, ntok, ev_q)
                for half in range(2):
                    wtile = load_w(3072 + half * 512, 512)

                    def ev_o(a, tt, half=half):
                        s = nxt(stf, "sf")
                        scaled_copy(evac_engine(), s.t[:], a.t[:], 1.0, [a], [s])
                        k.dma("act", o_d.t[tt * 128:(tt + 1) * 128, half * 512:(half + 1) * 512], s.t[:], r=[s], w=[o_d])
                    tm_cols(wtile, 512, ntiles, ev_o)
                for half in range(2):
                    wa = load_w(4112 + half * 512, 512)
                    wgl = load_w(4112 + 1024 + half * 512, 512)
                    for cc in range(4):
                        for b0 in range(0, ntok, 512):
                            pa = nxt(acc, "a")
                            pg = nxt(acc, "a")
                            for (pp, ww) in ((pa, wa), (pg, wgl)):
                                for dc in range(DC):
                                    k.op("pe", lambda e: e.matmul(pp.t[:], lhsT=ww.t[:, dc, cc * 128:(cc + 1) * 128],
                                                                  rhs=xnT.t[:, dc, b0:b0 + 512], start=(dc == 0), stop=(dc == DC - 1)),
                                         r=[xnT, ww], w=[pp])
                            sg = nxt(sig, "sg")
                            k.op("act", lambda e: e.activation(out=sg.t[:], in_=pg.t[:], func=AF.Sigmoid), r=[pg], w=[sg])
                            s = nxt(stb, "sb")
                            k.op("dve", lambda e: e.tensor_tensor(out=s.t[:], in0=pa.t[:], in1=sg.t[:], op=ALU.mult),
                                 r=[pa, sg], w=[s])
                            k.dma("act", uT_d.t[half * 4 + cc, :, b0:b0 + 512], s.t[:], r=[s], w=[uT_d])

            load_AB(1)
            build_xnT(ctxl, 0, 2, A_t, B_t)
            project(NCTX, 0, False)
            load_AB(0)
            build_xnT(xall, 0, 16, A_t, B_t)
            project(NOWN, 2, True)
            build_xnT(xall, NOWN, 16, A_t, B_t)
            project(NOWN, 18, False)
        k.end_phase()

        for j in range(NPRE):
            for (src, dst) in ((wg, wgb), (wu, wub)):
                for q in range(4):
                    rs = slice(q * (D // 4), (q + 1) * (D // 4))
                    k.dma("pool", dst.t[j, rs, :].rearrange("d (a b) -> d a b", b=RUN),
                          src.t[j, rs, :].rearrange("d (a b) -> d a b", b=RUN), r=[], w=[dst], sem="pre")
            for q in range(4):
                fq = FF // 4
                k.dma("pool", wdb.t[j, q * fq:(q + 1) * fq, :], wd.t[j, q * fq:(q + 1) * fq, :], r=[], w=[wdb], sem="pre")
        if NPRE:
            k.keep.add("pre")
            k.nosync.add("pre")
            for T_ in (wgb, wub, wdb):
                T_.persist = True

        with ExitStack() as es:
            gall = k.tile(es, "gall", [128, NCH, 16], F32)
            lf = k.tile(es, "lf", [128, NCH, 16], F32)
            k.dma("sp", gall.t[:], g_d.t.ap().rearrange("(c p) n -> p c n", p=128), w=[gall])
            k.op("act", lambda e: e.activation(out=lf.t[:], in_=gall.t[:], func=AF.Exp, scale=-1.0), r=[gall], w=[lf])
            k.op("act", lambda e: e.activation(out=lf.t[:], in_=lf.t[:], func=AF.Ln, bias=1.0), r=[lf], w=[lf])
            k.op("dve", lambda e: e.tensor_scalar(out=lf.t[:], in0=lf.t[:], scalar1=-1.0, scalar2=None, op0=ALU.mult),
                 r=[lf], w=[lf])
            NV = NCH * 4
            es_pro = ExitStack()
            pp1 = k.tile(es_pro, "pp1", [128, 512], F32, psum=True)
            pp2 = k.tile(es_pro, "pp2", [128, 512], F32, psum=True)
            wcol = [k.tile(es, f"wcol{d}", [128, NCH, 4], F32) for d in range(2)]
            wint = [k.tile(es, f"wint{d}", [128, NCH, 4], F32) for d in range(2)]
            clmp = [k.tile(es, f"clmp{d}", [128, NCH, 4], F32) for d in range(2)]
            acol = k.tile(es, "acol", [128, NCH, 4], F32)
            bcol = k.tile(es, "bcol", [128, NCH, 4], F32)
            grep = k.tile(es, "grep", [128, NCH, 4], F32)
            mmax = k.tile(es, "mmax", [128, NCH, 4], F32)
            Mp = k.tile(es, "Mp", [128, NCH, 4], F32)
            warg = k.tile(es, "warg", [128, NCH, 4], F32)
            mcur = k.tile(es, "mcur", [128, 4], F32)
            aT = k.tile(es, "aT", [128, 2, 128], F32)
            mx = k.tile(es, "mx", [128, 2], F32)
            dg = k.tile(es, "dg", [128, 136], F32)
            orders = [list(range(0, 18)), [1, 0] + list(range(33, 17, -1)) + list(range(17, 1, -1))]
            for d in range(2):
                tri = triF if d == 0 else triB
                i0, f0 = d * 8, d * 8 + 4
                k.op("pe", lambda e: e.matmul(pp1.t[:, 0:NV].rearrange("p (c h) -> p c h", h=4), lhsT=tri,
                                              rhs=lf.t[:, :, f0:f0 + 4], start=True, stop=True), r=[cst, lf], w=[pp1])
                k.op("dve", lambda e: e.tensor_copy(out=bcol.t[:], in_=pp1.t[:, 0:NV].rearrange("p (c h) -> p c h", h=4)),
                     r=[pp1], w=[bcol])
                k.op("dve", lambda e: e.tensor_tensor(out=acol.t[:], in0=gall.t[:, :, i0:i0 + 4], in1=bcol.t[:], op=ALU.subtract),
                     r=[gall, bcol], w=[acol])
                k.op("pe", lambda e: e.matmul(pp2.t[:, 0:NV].rearrange("p (c h) -> p c h", h=4), lhsT=ones,
                                              rhs=lf.t[:, :, f0:f0 + 4], start=True, stop=True), r=[cst, lf], w=[pp2])
                k.op("dve", lambda e: e.tensor_copy(out=grep.t[:], in_=pp2.t[:, 0:NV].rearrange("p (c h) -> p c h", h=4)),
                     r=[pp2], w=[grep])
                af = acol.t[:].rearrange("p c h -> p (c h)")
                k.op("pe", lambda e: e.transpose(pp1.t[:, 0:128], af[:, 0:128], ident), r=[acol, cst], w=[pp1])
                k.op("pe", lambda e: e.transpose(pp1.t[0:8, 128:256], af[:, 128:136], ident), r=[acol, cst], w=[pp1])
                k.op("dve", lambda e: e.reduce_max(out=mx.t[:, 0:1], in_=pp1.t[:, 0:128], axis=AX.X), r=[pp1], w=[mx])
                k.op("dve", lambda e: e.reduce_max(out=mx.t[0:8, 1:2], in_=pp1.t[0:8, 128:256], axis=AX.X), r=[pp1], w=[mx])
                k.op("dve", lambda e: e.tensor_scalar(out=dg.t[:, 0:128], in0=ident, scalar1=mx.t[:, 0:1], scalar2=None,
                                                      op0=ALU.mult), r=[cst, mx], w=[dg])
                k.op("dve", lambda e: e.tensor_scalar(out=dg.t[0:8, 128:136], in0=cst.t[0:8, 0:8], scalar1=mx.t[0:8, 1:2],
                                                      scalar2=None, op0=ALU.mult), r=[cst, mx], w=[dg])
                k.op("pe", lambda e: e.matmul(pp2.t[:, 0:128], lhsT=ones, rhs=dg.t[:, 0:128], start=True, stop=True),
                     r=[cst, dg], w=[pp2])
                k.op("pe", lambda e: e.matmul(pp2.t[:, 128:136], lhsT=cst.t[0:8, 384:512], rhs=dg.t[0:8, 128:136],
                                              start=True, stop=True), r=[cst, dg], w=[pp2])
                k.op("dve", lambda e: e.tensor_copy(out=mmax.t[:].rearrange("p c h -> p (c h)"), in_=pp2.t[:, 0:NV]),
                     r=[pp2], w=[mmax])
                k.op("dve", lambda e: e.memset(mcur.t[:], 0.0), w=[mcur])
                for c in orders[d]:
                    k.op("dve", lambda e: e.tensor_tensor(out=Mp.t[:, c, :], in0=mmax.t[:, c, :], in1=mcur.t[:], op=ALU.max),
                         r=[mmax, mcur], w=[Mp])
                    k.op("dve", lambda e: e.tensor_tensor(out=warg.t[:, c, :], in0=mcur.t[:], in1=Mp.t[:, c, :], op=ALU.subtract),
                         r=[mcur, Mp], w=[warg])
                    k.op("dve", lambda e: e.tensor_tensor(out=mcur.t[:], in0=grep.t[:, c, :], in1=Mp.t[:, c, :], op=ALU.add),
                         r=[grep, Mp], w=[mcur])
                k.op("dve", lambda e: e.tensor_tensor(out=acol.t[:], in0=acol.t[:], in1=Mp.t[:], op=ALU.subtract),
                     r=[acol, Mp], w=[acol])
                k.op("act", lambda e: e.activation(out=wcol[d].t[:], in_=acol.t[:], func=AF.Exp), r=[acol], w=[wcol[d]])
                k.op("act", lambda e: e.activation(out=wint[d].t[:], in_=warg.t[:], func=AF.Exp), r=[warg], w=[wint[d]])
                k.op("dve", lambda e: e.tensor_tensor(out=bcol.t[:], in0=bcol.t[:], in1=Mp.t[:], op=ALU.add),
                     r=[bcol, Mp], w=[bcol])
                k.op("act", lambda e: e.activation(out=clmp[d].t[:], in_=bcol.t[:], func=AF.Exp, scale=-1.0),
                     r=[bcol], w=[clmp[d]])

            k.sync_all()
            es_pro.close()
            CxD = [[k.tile(es, f"Cx{d}_{h}", [128, 2, 257], F32) for h in range(NH)] for d in range(2)]
            Cb = [k.tile(es, f"Cb{h}", [128, 2, 257], BF16) for h in range(NH)]
            kt = [k.tile(es, f"kt{i}", [128, 8, 128], BF16) for i in range(2)]
            qt = [k.tile(es, f"qt{i}", [128, 8, 128], BF16) for i in range(2)]
            kk = [k.tile(es, f"kk{i}", [128, W], BF16) for i in range(2)]
            vv = [k.tile(es, f"vv{i}", [128, NH, 257], BF16) for i in range(2)]
            vx = [k.tile(es, f"vx{i}", [128, 257], BF16) for i in range(2)]
            sT = [k.tile(es, f"sT{i}", [128, 128], BF16) for i in range(2)]
            STp1 = k.tile(es, "STp", [128, 2, 128], F32, psum=True)
            STp = [STp1, STp1]
            Pp = [k.tile(es, f"Pp{i}", [128, 257], F32, psum=True) for i in range(2)]
            Cnps = [k.tile(es, f"Cnp{i}", [128, 2, 512], F32, psum=True) for i in range(2)]
            dens = [k.tile(es, f"den{i}", [128, 2], F32) for i in range(NH)]
            hh = [k.tile(es, f"hh{i}", [128, W], F32) for i in range(2)]
            h1 = k.tile(es, "h1", [128, W], F32)
            oo = k.tile(es, "oo", [128, W], F32)
            mw = k.tile(es, "mw", [128, W], F32)
            st6s = [k.tile(es, f"st6_{i}", [128, 6], F32) for i in range(NH)]
            mvs = [k.tile(es, f"mv{i}", [128, 2], F32) for i in range(NH)]
            mlb = k.tile(es, "mlb", [128, W], BF16)
            mst = k.tile(es, "mst", [128, 8, 128], BF16)
            tp2 = k.tile(es, "tp2", [128, 4, 128], BF16, psum=True)
            load_bc(mw, mnw, 0, W)
            for i in range(2):
                k.op("pool", lambda e: e.memset(vv[i].t[:, :, 256:257], 1.0), w=[vv[i]])
            step = 0
            for d in range(2):
                for h in range(NH):
                    k.op("pool", lambda e: e.memset(CxD[d][h].t[:], 0.0), w=[CxD[d][h]])
            sched = []
            for n_ in range(18):
                sched.append((0, orders[0][n_]))
                sched.append((1, orders[1][n_]))
            sched += [(1, c_) for c_ in orders[1][18:]]
            synced = False
            for (d, c) in sched:
                if True:
                    maskb = maskFb if d == 0 else maskBb
                    Cx = CxD[d]
                    full = 2 <= c < 18
                    if d == 1 and full and not synced:
                        synced = True
                        k.sync_all()
                    i = step % 2
                    step += 1
                    k.dma("sp", kt[i].t[:], kT_d.t[c].rearrange("p (a t) -> p a t", t=128), w=[kt[i]])
                    k.dma("sp", kk[i].t[:], k_d.t[c * 128:(c + 1) * 128, :], w=[kk[i]])
                    k.dma("sp", vv[i].t[:, :, 0:256], v_d.t[c * 128:(c + 1) * 128, :].rearrange("p (h e) -> p h e", e=256),
                          w=[vv[i]])
                    if full:
                        k.dma("sp", qt[i].t[:], qT_d.t[c - 2].rearrange("p (a t) -> p a t", t=128), w=[qt[i]])
                        if d == 1:
                            k.dma("sp", h1.t[:], h1_d.t[(c - 2) * 128:(c - 1) * 128, :], w=[h1])
                            k.dma("sp", oo.t[:], o_d.t[(c - 2) * 128:(c - 1) * 128, :], w=[oo])
                    hcur = hh[i]
                    for h in range(NH):
                        wc = wcol[d].t[:, c, h:h + 1]
                        wi = wint[d].t[:, c, h:h + 1]
                        cl = clmp[d].t[:, c, h:h + 1]
                        vxt = vx[h % 2]
                        den = dens[h]
                        Cnp = Cnps[h % 2]
                        k.op("act", lambda e: e.activation(out=vxt.t[:], in_=vv[i].t[:, h, :], func=AF.Copy, scale=wc),
                             r=[vv[i], wcol[d]], w=[vxt])
                        if full:
                            k.op("act", lambda e: e.activation(out=Cb[h].t[:], in_=Cx[h].t[:], func=AF.Copy, scale=wi),
                                 r=[Cx[h], wint[d]], w=[Cb[h]])
                            sp_ = STp[h % 2]
                            for dc in range(2):
                                k.op("pe", lambda e: e.matmul(sp_.t[:, h % 2, :], lhsT=kt[i].t[:, h * 2 + dc, :], rhs=qt[i].t[:, h * 2 + dc, :],
                                                              start=(dc == 0), stop=(dc == 1)), r=[kt[i], qt[i]], w=[sp_])
                            s_ = sT[h % 2]
                            k.op("dve", lambda e: e.tensor_tensor(out=s_.t[:], in0=sp_.t[:, h % 2, :], in1=maskb, op=ALU.mult),
                                 r=[sp_, cbf], w=[s_])
                            p_ = Pp[h % 2]
                            k.op("pe", lambda e: e.matmul(p_.t[:], lhsT=s_.t[:], rhs=vxt.t[:], start=True, stop=False),
                                 r=[s_, vxt], w=[p_])
                            for dc in range(2):
                                k.op("pe", lambda e: e.matmul(p_.t[:], lhsT=qt[i].t[:, h * 2 + dc, :], rhs=Cb[h].t[:, dc, :],
                                                              start=False, stop=(dc == 1)), r=[qt[i], Cb[h]], w=[p_])
                            k.op("act", lambda e: e.activation(out=den.t[:, 0:1], in_=p_.t[:, 256:257], func=AF.Abs), r=[p_], w=[den])
                            k.op("dve", lambda e: e.tensor_scalar(out=den.t[:, 0:1], in0=den.t[:, 0:1], scalar1=cl, scalar2=None,
                                                                  op0=ALU.max), r=[den, clmp[d]], w=[den])
                            k.op("dve", lambda e: e.reciprocal(out=den.t[:, 1:2], in_=den.t[:, 0:1]), r=[den], w=[den])
                            k.op("act", lambda e: e.activation(out=hcur.t[:, h * 256:(h + 1) * 256], in_=p_.t[:, 0:256],
                                                               func=AF.Copy, scale=den.t[:, 1:2]), r=[p_, den], w=[hcur])
                        for dc in range(2):
                            k.op("pe", lambda e: e.matmul(Cnp.t[:, dc, 0:257], lhsT=kk[i].t[:, h * 256 + dc * 128: h * 256 + (dc + 1) * 128],
                                                          rhs=vxt.t[:], start=True, stop=True), r=[kk[i], vxt], w=[Cnp])
                        k.op("dve", lambda e: e.scalar_tensor_tensor(out=Cx[h].t[:], in0=Cx[h].t[:], scalar=wi, in1=Cnp.t[:, :, 0:257],
                                                                     op0=ALU.mult, op1=ALU.add), r=[Cx[h], wint[d], Cnp], w=[Cx[h]])
                    if not full:
                        continue
                    r0 = (c - 2) * 128
                    if d == 0:
                        k.dma("act", h1_d.t[r0:r0 + 128, :], hcur.t[:], r=[hcur], w=[h1_d])
                        continue
                    k.op("dve", lambda e: e.tensor_tensor(out=hcur.t[:], in0=hcur.t[:], in1=h1.t[:], op=ALU.add),
                         r=[hcur, h1], w=[hcur])
                    k.op("act", lambda e: e.activation(out=oo.t[:], in_=oo.t[:], func=AF.Sigmoid), r=[oo], w=[oo])
                    for h in range(NH):
                        hs = hcur.t[:, h * 256:(h + 1) * 256]
                        st6 = st6s[h]
                        mv = mvs[h]
                        k.op("dve", lambda e: e.bn_stats(out=st6.t[:], in_=hs), r=[hcur], w=[st6])
                        k.op("dve", lambda e: e.bn_aggr(out=mv.t[:], in_=st6.t[:]), r=[st6], w=[mv])
                        k.op("act", lambda e: e.activation(out=mv.t[:, 1:2], in_=mv.t[:, 1:2], func=AF.Sqrt, bias=epsb.t[:, 0:1]),
                             r=[mv, epsb], w=[mv])
                        k.op("dve", lambda e: e.reciprocal(out=mv.t[:, 1:2], in_=mv.t[:, 1:2]), r=[mv], w=[mv])
                        k.op("dve", lambda e: e.tensor_scalar(out=hs, in0=hs, scalar1=mv.t[:, 0:1], scalar2=mv.t[:, 1:2],
                                                              op0=ALU.subtract, op1=ALU.mult), r=[hcur, mv], w=[hcur])
                    k.op("dve", lambda e: e.tensor_tensor(out=hcur.t[:], in0=hcur.t[:], in1=oo.t[:], op=ALU.mult),
                         r=[hcur, oo], w=[hcur])
                    k.op("dve", lambda e: e.tensor_tensor(out=mlb.t[:], in0=hcur.t[:], in1=mw.t[:], op=ALU.mult),
                         r=[hcur, mw], w=[mlb])
                    for gq in range(2):
                        for q in range(4):
                            fc = gq * 4 + q
                            k.op("pe", lambda e: e.transpose(tp2.t[:, q, :], mlb.t[:, fc * 128:(fc + 1) * 128], identb),
                                 r=[mlb, cbf], w=[tp2])
                        k.op("act", lambda e: e.copy(out=mst.t[:, gq * 4:(gq + 1) * 4, :], in_=tp2.t[:]), r=[tp2], w=[mst])
                    k.dma("act", mixT_d.t[0:8, :, r0:r0 + 128].rearrange("f p t -> p f t"), mst.t[:], r=[mst], w=[mixT_d])
        k.end_phase()

        with ExitStack() as es:
            cw = k.tile(es, "cw", [128, 8, KT], F32)
            cbv = k.tile(es, "cbv", [128, 8], F32)
            lw = k.tile(es, "lw", [128, 8], F32)
            lb = k.tile(es, "lb", [128, 8], F32)
            for tl, src in ((cw, convw), (cbv, convb), (lw, cnw), (lb, cnb)):
                k.dma("sp", tl.t[:], src.t.ap(), w=[tl])
            TB = 1024
            dgs = k.tile(es, "dgs", [128, 8 * KT, 128], BF16)
            for cc in range(8):
                for t in range(KT):
                    k.op("dve", lambda e: e.tensor_scalar(out=dgs.t[:, cc * KT + t, :], in0=identb, scalar1=cw.t[:, cc, t:t + 1],
                                                          scalar2=None, op0=ALU.mult), r=[cbf, cw], w=[dgs])
            u = k.tile(es, "u", [128, 8, TB], BF16)
            ycs = [k.tile(es, f"yc{cc}", [128, TB], F32) for cc in range(8)]
            sq = k.tile(es, "sq", [128, TB], F32)
            yps = [k.tile(es, f"yps{i}", [128, TB], F32, psum=True) for i in range(2)]
            sps = [k.tile(es, f"sps{i}", [128, 512], F32, psum=True) for i in range(4)]
            mean = k.tile(es, "mean", [128, TB], F32)
            rstd = k.tile(es, "rstd", [128, TB], F32)
            mo = [k.tile(es, f"mo{i}", [128, TB], BF16) for i in range(2)]
            taps = [15] + [t for t in range(KT) if t != 15]
            for blk in range(NOWN // TB):
                t0 = blk * TB
                k.dma("sp", u.t[:], uT_d.t[:, :, t0:t0 + TB].rearrange("c p t -> p c t"), w=[u])
                for cc in range(8):
                    y = ycs[cc]
                    yp = yps[cc % 2]
                    for hf in range(TB // 512):
                        y3 = yp.t[:, hf * 512:(hf + 1) * 512].rearrange("p (r t) -> p r t", t=64)
                        u3 = u.t[:, cc, hf * 512:(hf + 1) * 512].rearrange("p (r t) -> p r t", t=64)
                        for ti, t in enumerate(taps):
                            o = t - 15
                            lo, hi = max(0, -o), 64 - max(0, o)
                            k.op("pe", lambda e: e.matmul(y3[:, :, lo:hi], lhsT=dgs.t[:, cc * KT + t, :], rhs=u3[:, :, lo + o:hi + o],
                                                          start=(ti == 0), stop=(ti == KT - 1), skip_group_check=True),
                                 r=[dgs, u], w=[yp])
                    k.op("act", lambda e: e.activation(out=y.t[:], in_=yp.t[:], func=AF.Identity, bias=cbv.t[:, cc:cc + 1]),
                         r=[yp, cbv], w=[y])
                for cc in range(8):
                    y = ycs[cc]
                    k.op("act", lambda e: e.activation(out=sq.t[:], in_=y.t[:], func=AF.Square), r=[y], w=[sq])
                    for hf in range(2):
                        k.op("pe", lambda e: e.matmul(sps[hf].t[:], lhsT=ones, rhs=y.t[:, hf * 512:(hf + 1) * 512],
                                                      start=(cc == 0), stop=(cc == 7)), r=[cst, y], w=[sps[hf]])
                        k.op("pe", lambda e: e.matmul(sps[2 + hf].t[:], lhsT=ones, rhs=sq.t[:, hf * 512:(hf + 1) * 512],
                                                      start=(cc == 0), stop=(cc == 7)), r=[cst, sq], w=[sps[2 + hf]])
                for hf in range(2):
                    sl = slice(hf * 512, (hf + 1) * 512)
                    k.op("dve", lambda e: e.tensor_scalar(out=mean.t[:, sl], in0=sps[hf].t[:], scalar1=1.0 / CW, scalar2=None,
                                                          op0=ALU.mult), r=[sps[hf]], w=[mean])
                    k.op("dve", lambda e: e.tensor_tensor(out=sq.t[:, sl], in0=mean.t[:, sl], in1=mean.t[:, sl], op=ALU.mult),
                         r=[mean], w=[sq])
                    k.op("dve", lambda e: e.scalar_tensor_tensor(out=rstd.t[:, sl], in0=sps[2 + hf].t[:], scalar=1.0 / CW,
                                                                 in1=sq.t[:, sl], op0=ALU.mult, op1=ALU.subtract),
                         r=[sps[2 + hf], sq], w=[rstd])
                    k.op("act", lambda e: e.activation(out=rstd.t[:, sl], in_=rstd.t[:, sl], func=AF.Sqrt, bias=epsb.t[:, 0:1]),
                         r=[rstd, epsb], w=[rstd])
                    k.op("dve", lambda e: e.reciprocal(out=rstd.t[:, sl], in_=rstd.t[:, sl]), r=[rstd], w=[rstd])
                for cc in range(8):
                    y = ycs[cc]
                    en = "dve" if cc % 2 == 0 else "pool"
                    k.op(en, lambda e: e.tensor_tensor(out=y.t[:], in0=y.t[:], in1=mean.t[:], op=ALU.subtract),
                         r=[y, mean], w=[y])
                    k.op(en, lambda e: e.tensor_tensor(out=y.t[:], in0=y.t[:], in1=rstd.t[:], op=ALU.mult),
                         r=[y, rstd], w=[y])
                    m = mo[cc % 2]
                    k.op("act", lambda e: e.activation(out=m.t[:], in_=y.t[:], func=AF.Silu, bias=lb.t[:, cc:cc + 1],
                                                       scale=lw.t[:, cc:cc + 1]), r=[y, lw, lb], w=[m])
                    k.dma("act", mixT_d.t[8 + cc, :, t0:t0 + TB], m.t[:], r=[m], w=[mixT_d])
        k.end_phase()

        with ExitStack() as es:
            A2 = k.tile(es, "A2", [128, D], F32)
            B2 = k.tile(es, "B2", [128, D], F32)
            wo = k.tile(es, "wo", [128, DC, D], BF16)
            wr = k.tile(es, "wr", [128, DC, 16], F32)
            es_pre = ExitStack()
            g1b = k.tile(es_pre, "g1b", [128, D], F32)
            nw2 = k.tile(es_pre, "nw2", [128, D], F32)
            wof = [k.tile(es_pre, f"wof{i}", [128, D], F32) for i in range(2)]
            load_bc(g1b, ada_d, 2 * D)
            load_bc(B2, ada_d, 3 * D)
            load_bc(A2, ada_d, 4 * D)
            load_bc(nw2, n2w, 0)
            k.dma("sp", wr.t[:], w_r.t.ap(), w=[wr])
            k.op("dve", lambda e: e.scalar_tensor_tensor(out=A2.t[:], in0=A2.t[:], scalar=1.0, in1=nw2.t[:],
                                                         op0=ALU.add, op1=ALU.mult), r=[A2, nw2], w=[A2])
            for fc in range(DC):
                wf = wof[fc % 2]
                k.dma("sp", wf.t[:], w_out.t[fc * 128:(fc + 1) * 128, :], w=[wf])
                k.op("dve", lambda e: e.tensor_tensor(out=wo.t[:, fc, :], in0=wf.t[:], in1=g1b.t[:], op=ALU.mult),
                     r=[wf, g1b], w=[wo])
            k.sync_all()
            es_pre.close()
            mt = [k.tile(es, f"mt{i}", [128, DC, 512], BF16) for i in range(2)]
            xt = [k.tile(es, f"x4_{i}", [128, D], F32) for i in range(2)]
            x1 = [k.tile(es, f"x1_{i}", [128, D], F32) for i in range(2)]
            junk = k.tile(es, "junk4", [128, D], BF16)
            xn2s = [k.tile(es, f"xn2_{i}", [128, D], F32) for i in range(2)]
            pend4 = []
            xn2b = [k.tile(es, f"xn2b{i}", [128, D], BF16) for i in range(2)]
            xT = k.tile(es, "xT", [128, DC, 128], F32)
            ss = k.tile(es, "ss4", [128, 2], F32)
            pacc = [k.tile(es, f"pacc{i}", [128, 512], F32, psum=True) for i in range(3)]
            ptr = [k.tile(es, f"ptr{i}", [128, 4, 128], F32, psum=True) for i in range(2)]
            plg = k.tile(es, "plg", [128, 16], F32, psum=True)
            sm = k.tile(es, "sm", [128, 4], F32)
            ex = k.tile(es, "ex", [128, 16], F32)
            afft = [k.tile(es, f"afft{i}", [128, 32], F32) for i in range(2)]
            na = 0
            for tb in range(NOWN // 512):
                m = mt[tb % 2]
                k.dma("sp", m.t[:], mixT_d.t[:, :, tb * 512:(tb + 1) * 512].rearrange("f p t -> p f t"), w=[m])
                for t4 in range(4):
                    tt = tb * 4 + t4
                    x = xt[tt % 2]
                    x1t = x1[tt % 2]
                    xn2 = xn2s[tt % 2]
                    k.dma("sp", x.t[:], xall.t[tt * 128:(tt + 1) * 128, :], w=[x])
                    for nb in range(4):
                        a = pacc[na % 3]
                        na += 1
                        for fc in range(DC):
                            k.op("pe", lambda e: e.matmul(a.t[:], lhsT=m.t[:, fc, t4 * 128:(t4 + 1) * 128],
                                                          rhs=wo.t[:, fc, nb * 512:(nb + 1) * 512], start=(fc == 0), stop=(fc == DC - 1)),
                                 r=[m, wo], w=[a])
                        k.op("dve", lambda e: e.tensor_tensor(out=x1t.t[:, nb * 512:(nb + 1) * 512], in0=a.t[:],
                                                              in1=x.t[:, nb * 512:(nb + 1) * 512], op=ALU.add), r=[a, x], w=[x1t])
                    while len(pend4) > 0 and pend4[0].__defaults__[0] < tt:
                        pend4.pop(0)()
                    k.dma("act", x1_d.t[tt * 128:(tt + 1) * 128, :], x1t.t[:], r=[x1t], w=[x1_d])
                    k.op("act", lambda e: e.activation(out=junk.t[:], in_=x1t.t[:], func=AF.Square, accum_out=ss.t[:, 0:1]),
                         r=[x1t], w=[junk, ss])
                    k.op("act", lambda e: e.activation(out=ss.t[:, 1:2], in_=ss.t[:, 0:1], func=AF.Sqrt, scale=1.0 / D, bias=epsb.t[:, 0:1]),
                         r=[ss, epsb], w=[ss])
                    k.op("dve", lambda e: e.reciprocal(out=ss.t[:, 1:2], in_=ss.t[:, 1:2]), r=[ss], w=[ss])
                    k.op("dve", lambda e: e.scalar_tensor_tensor(out=xn2.t[:], in0=x1t.t[:], scalar=ss.t[:, 1:2], in1=A2.t[:],
                                                                 op0=ALU.mult, op1=ALU.mult), r=[x1t, ss, A2], w=[xn2])
                    k.op("dve", lambda e: e.tensor_tensor(out=xn2.t[:], in0=xn2.t[:], in1=B2.t[:], op=ALU.add),
                         r=[xn2, B2], w=[xn2])
                    xb = xn2b[tt % 2]
                    k.op("act", lambda e: e.copy(out=xb.t[:], in_=xn2.t[:]), r=[xn2], w=[xb])
                    k.idma(out=xn2_sh.t[:, :], in_=xb.t[:, :], out_off=idx_own.t[:, tt:tt + 1], bounds=4095,
                           r=[xb, idx_own], w=[xn2_sh])
                    def router(tt=tt, xn2=xn2):
                        for gq in range(4):
                            tp = ptr[gq % 2]
                            for q in range(4):
                                dc = gq * 4 + q
                                k.op("pe", lambda e: e.transpose(tp.t[:, q, :], xn2.t[:, dc * 128:(dc + 1) * 128], ident),
                                     r=[xn2, cst], w=[tp])
                            if gq % 2 == 0:
                                k.op("act", lambda e: e.copy(out=xT.t[:, gq * 4:(gq + 1) * 4, :], in_=tp.t[:]), r=[tp], w=[xT])
                            else:
                                k.op("dve", lambda e: e.tensor_copy(out=xT.t[:, gq * 4:(gq + 1) * 4, :], in_=tp.t[:]), r=[tp], w=[xT])
                        for dc in range(DC):
                            k.op("pe", lambda e: e.matmul(plg.t[:], lhsT=xT.t[:, dc, :], rhs=wr.t[:, dc, :], start=(dc == 0),
                                                          stop=(dc == DC - 1)), r=[xT, wr], w=[plg])
                        k.op("dve", lambda e: e.reduce_max(out=sm.t[:, 0:1], in_=plg.t[:], axis=AX.X), r=[plg], w=[sm])
                        k.op("dve", lambda e: e.tensor_scalar(out=sm.t[:, 1:2], in0=sm.t[:, 0:1], scalar1=-1.0, scalar2=None,
                                                              op0=ALU.mult), r=[sm], w=[sm])
                        k.op("act", lambda e: e.activation(out=ex.t[:], in_=plg.t[:], func=AF.Exp, bias=sm.t[:, 1:2],
                                                           accum_out=sm.t[:, 2:3]), r=[plg, sm], w=[ex, sm])
                        k.op("dve", lambda e: e.reciprocal(out=sm.t[:, 3:4], in_=sm.t[:, 2:3]), r=[sm], w=[sm])
                        af = afft[tt % 2]
                        k.op("dve", lambda e: e.tensor_scalar(out=af.t[:, 0:16], in0=ex.t[:], scalar1=sm.t[:, 3:4], scalar2=None,
                                                              op0=ALU.mult), r=[ex, sm], w=[af])
                        k.op("dve", lambda e: e.tensor_copy(out=af.t[:, 16:24], in_=af.t[:, 8:16]), r=[af], w=[af])
                        k.op("dve", lambda e: e.tensor_copy(out=af.t[:, 24:32], in_=af.t[:, 0:8]), r=[af], w=[af])
                        k.idma(out=aff_sh.t[:, :], in_=af.t[:, 0:16], out_off=idx_own.t[:, tt:tt + 1], bounds=8191,
                               r=[af, idx_own], w=[aff_sh])
                        k.idma(out=aff_sh.t[:, :], in_=af.t[:, 16:32], out_off=idx_own2.t[:, tt:tt + 1], bounds=8191,
                               r=[af, idx_own2], w=[aff_sh])
                    pend4.append(router)
            while pend4:
                pend4.pop(0)()
        k.pair_barrier(bsrc, bdst)
        if dbg:
            k.dma("sp", dbg_x1.t.ap(), x1_d.t.ap(), r=[x1_d], w=[dbg_x1])
            k.dma("sp", dbg_mix.t.ap(), mixT_d.t.ap(), r=[mixT_d], w=[dbg_mix])
            k.dma("sp", dbg_aff.t.ap(), aff_sh.t.ap(), r=[aff_sh], w=[dbg_aff])

        def tok_scatter(j, t):
            k.idma(out=tokg_d[j].t[:, :], in_=pk.t[:, t, j, :], out_off=pidx.t[:, t, j:j + 1], bounds=CAP - 1,
                   r=[pk, pidx], w=[tokg_d[j]], sem=f"tk{j}")

        with ExitStack() as es:
            affa = k.tile(es, "affa", [128, 32, 16], F32)
            affo = k.tile(es, "affo", [128, 32, 8], F32)
            for t in range(32):
                k.idma(out=affa.t[:, t, :], in_=aff_sh.t[:, :], in_off=idx_aff.t[:, t:t + 1], bounds=8191,
                       r=[aff_sh, idx_aff], w=[affa])
            k.op("dve", lambda e: e.tensor_copy(out=affo.t[:], in_=affa.t[:, :, 0:8]), r=[affa], w=[affo])
            lo = k.tile(es, "lo", [128, 8], F32)
            mid = k.tile(es, "mid", [128, 8], F32)
            cntt = k.tile(es, "cntt", [128, 8], F32)
            stp = k.tile(es, "stp", [128, 8], F32)
            cmp = k.tile(es, "cmp", [128, 32], F32)
            tot = k.tile(es, "tot", [128, 8], F32, psum=True)
            k.op("dve", lambda e: e.memset(lo.t[:], 0.0), w=[lo])
            for it in range(1, NBIS + 1):
                dk = 2.0 ** (-it)
                k.op("dve", lambda e: e.tensor_scalar(out=mid.t[:], in0=lo.t[:], scalar1=dk, scalar2=None, op0=ALU.add),
                     r=[lo], w=[mid])
                for j in range(EL):
                    k.op("dve", lambda e: e.tensor_scalar(out=cmp.t[:], in0=affo.t[:, :, j], scalar1=mid.t[:, j:j + 1], scalar2=0.0,
                                                          op0=ALU.is_ge, op1=ALU.add, accum_out=cntt.t[:, j:j + 1]),
                         r=[affo, mid], w=[cmp, cntt])
                k.op("pe", lambda e: e.matmul(tot.t[:], lhsT=ones, rhs=cntt.t[:], start=True, stop=True), r=[cst, cntt], w=[tot])
                k.op("dve", lambda e: e.tensor_scalar(out=stp.t[:], in0=tot.t[:], scalar1=CAP - 0.5, scalar2=dk,
                                                      op0=ALU.is_ge, op1=ALU.mult), r=[tot], w=[stp])
                k.op("dve", lambda e: e.tensor_tensor(out=lo.t[:], in0=lo.t[:], in1=stp.t[:], op=ALU.add), r=[lo, stp], w=[lo])
            mask = k.tile(es, "mask", [128, 32, 8], F32)
            for j in range(EL):
                k.op("dve", lambda e: e.tensor_scalar(out=mask.t[:, :, j], in0=affo.t[:, :, j], scalar1=lo.t[:, j:j + 1], scalar2=None,
                                                      op0=ALU.is_ge), r=[affo, lo], w=[mask])
            ppos = k.tile(es, "ppos", [128, 256], F32, psum=True)
            pcnt = k.tile(es, "pcnt", [128, 256], F32, psum=True)
            mflat = mask.t[:].rearrange("p t j -> p (t j)")
            k.op("pe", lambda e: e.matmul(ppos.t[:], lhsT=strictL, rhs=mflat, start=True, stop=True), r=[cst, mask], w=[ppos])
            k.op("pe", lambda e: e.matmul(pcnt.t[:], lhsT=ones, rhs=mflat, start=True, stop=True), r=[cst, mask], w=[pcnt])
            sA = k.tile(es, "sA", [128, 32, 8], F32)
            sB = k.tile(es, "sB", [128, 32, 8], F32)
            cn0 = k.tile(es, "cn0", [128, 32, 8], F32)
            k.op("dve", lambda e: e.tensor_copy(out=cn0.t[:].rearrange("p t j -> p (t j)"), in_=pcnt.t[:]), r=[pcnt], w=[cn0])
            k.op("dve", lambda e: e.tensor_copy(out=sA.t[:], in_=cn0.t[:]), r=[cn0], w=[sA])
            a_, b_ = sA, sB
            for s in (1, 2, 4, 8, 16):
                k.op("dve", lambda e: e.tensor_copy(out=b_.t[:], in_=a_.t[:]), r=[a_], w=[b_])
                k.op("dve", lambda e: e.tensor_tensor(out=b_.t[:, s:, :], in0=a_.t[:, s:, :], in1=a_.t[:, :32 - s, :], op=ALU.add),
                     r=[a_], w=[b_])
                a_, b_ = b_, a_
            pos = k.tile(es, "pos", [128, 32, 8], F32)
            k.op("dve", lambda e: e.tensor_tensor(out=pos.t[:], in0=a_.t[:], in1=cn0.t[:], op=ALU.subtract), r=[a_, cn0], w=[pos])
            k.op("dve", lambda e: e.tensor_tensor(out=pos.t[:].rearrange("p t j -> p (t j)"), in0=pos.t[:].rearrange("p t j -> p (t j)"),
                                                  in1=ppos.t[:], op=ALU.add), r=[pos, ppos], w=[pos])
            val = k.tile(es, "val", [128, 32, 8], F32)
            k.op("dve", lambda e: e.tensor_scalar(out=val.t[:], in0=pos.t[:], scalar1=float(CAP) - 0.5, scalar2=None, op0=ALU.is_lt),
                 r=[pos], w=[val])
            k.op("dve", lambda e: e.tensor_tensor(out=val.t[:], in0=val.t[:], in1=mask.t[:], op=ALU.mult), r=[val, mask], w=[val])
            k.op("dve", lambda e: e.scalar_tensor_tensor(out=pos.t[:], in0=pos.t[:], scalar=-4096.0, in1=val.t[:],
                                                         op0=ALU.add, op1=ALU.mult), r=[pos, val], w=[pos])
            k.op("dve", lambda e: e.tensor_scalar(out=pos.t[:], in0=pos.t[:], scalar1=4096.0, scalar2=None, op0=ALU.add),
                 r=[pos], w=[pos])
            k.op("dve", lambda e: e.tensor_copy(out=pidx.t[:], in_=pos.t[:]), r=[pos], w=[pidx])
            for j in range(EL):
                k.op("dve", lambda e: e.tensor_copy(out=pk.t[:, :, j, 0], in_=gidt.t[:]), r=[gidt], w=[pk])
            k.op("dve", lambda e: e.tensor_copy(out=pk.t[:, :, :, 1], in_=affo.t[:]), r=[affo], w=[pk])
            for t in range(32):
                tok_scatter(0, t)
        k.end_phase()

        with ExitStack() as es:
            XeTs = [k.tile(es, f"XeT{i}", [128, DC, CAP], BF16) for i in range(2)]
            HT = k.tile(es, "HT", [128, NFC, CAP], BF16)
            wq = [k.tile(es, f"wq{i}", [128, DC * 512], BF16) for i in range(4)]
            xe = [k.tile(es, f"xe{i}", [128, D], BF16) for i in range(4)]
            tg = [[k.tile(es, f"tg{p}_{i}", [128, 2], F32) for i in range(4)] for p in range(2)]
            tgi = [[k.tile(es, f"tgi{p}_{i}", [128, 1], I32) for i in range(4)] for p in range(2)]
            yr = [[k.tile(es, f"yr{p}_{i}", [128, 2], F32) for i in range(4)] for p in range(2)]
            yri = [[k.tile(es, f"yri{p}_{i}", [128, 1], I32) for i in range(4)] for p in range(2)]
            yst = [k.tile(es, f"yst{i}", [128, D], F32) for i in range(4)]
            sg = [k.tile(es, f"sg6_{i}", [128, 512], F32) for i in range(2)]
            pb = [k.tile(es, f"pb{i}", [128, 512], F32, psum=True) for i in range(8)]
            tpp = [pb[6], pb[7]]
            tppv = [pb[i].t[:].bitcast(BF16)[:, 0:512].rearrange("p (q t) -> p q t", t=128) for i in (6, 7)]
            cnt6 = {"wn": 0}

            def prep_gather(j):
                p = j % 2
                for st in range(4):
                    k.dma("sp", tg[p][st].t[:], tokg_d[j].t[st * 128:(st + 1) * 128, :], r=[tokg_d[j]], w=[tg[p][st]])
                    x = xe[st]
                    k.op("dve", lambda e: e.tensor_copy(out=tgi[p][st].t[:], in_=tg[p][st].t[:, 0:1]), r=[tg[p][st]], w=[tgi[p][st]])
                    k.idma(out=x.t[:, :], in_=xn2_sh.t[:, :], in_off=tgi[p][st].t[:, 0:1], bounds=4095,
                           r=[xn2_sh, tgi[p][st]], w=[x])
                    k.op("dve", lambda e: e.tensor_tensor(out=yr[p][st].t[:, 1:2], in0=tg[p][st].t[:, 0:1], in1=ybt.t[:], op=ALU.add),
                         r=[tg[p][st], ybt], w=[yr[p][st]])
                    k.op("dve", lambda e: e.tensor_copy(out=yri[p][st].t[:], in_=yr[p][st].t[:, 1:2]), r=[yr[p][st]], w=[yri[p][st]])

            def prep_transpose(j):
                XeT = XeTs[j % 2]
                for st in range(4):
                    x = xe[st]
                    for gq in range(4):
                        tp = tpp[gq % 2]
                        tv_ = tppv[gq % 2]
                        for q in range(4):
                            dc = gq * 4 + q
                            k.op("pe", lambda e: e.transpose(tv_[:, q, :], x.t[:, dc * 128:(dc + 1) * 128], identb),
                                 r=[x, cbf], w=[tp])
                        if gq % 2 == 0:
                            k.op("act", lambda e: e.copy(out=XeT.t[:, gq * 4:(gq + 1) * 4, st * 128:(st + 1) * 128], in_=tv_),
                                 r=[tp], w=[XeT])
                        else:
                            k.op("dve", lambda e: e.tensor_copy(out=XeT.t[:, gq * 4:(gq + 1) * 4, st * 128:(st + 1) * 128], in_=tv_),
                                 r=[tp], w=[XeT])

            prep_gather(0)
            prep_transpose(0)
            gathered = {0}
            deferred = []
            for j in range(EL):
                p = j % 2
                XeT = XeTs[p]
                pend = [(j + 1, t) for t in range(32)] if j + 1 < EL else []
                pre = j < NPRE
                wq_eng = "sp" if pre else "pool"
                wsrc = [wgb, wub, wdb] if pre else []
                wgv = (wgb if pre else wg).t[j].rearrange("(dc p) f -> p dc f", p=128)
                wuv = (wub if pre else wu).t[j].rearrange("(dc p) f -> p dc f", p=128)
                for gi, f0 in enumerate(range(0, FF, 512)):
                    nf = min(512, FF - f0)
                    wn = cnt6["wn"]
                    tG = wq[wn % 4]
                    tU = wq[(wn + 1) % 4]
                    k.dma(wq_eng, tG.t[:, 0:DC * nf].rearrange("p (dc f) -> p dc f", f=nf), wgv[:, :, f0:f0 + nf], r=wsrc, w=[tG])
                    k.dma(wq_eng, tU.t[:, 0:DC * nf].rearrange("p (dc f) -> p dc f", f=nf), wuv[:, :, f0:f0 + nf], r=wsrc, w=[tU])
                    cnt6["wn"] += 2
                    if gi == 1:
                        while deferred:
                            deferred.pop(0)()
                    for _ in range(8):
                        if pend:
                            tok_scatter(*pend.pop(0))
                    if j + 1 < EL and not pend and (j + 1) not in gathered:
                        gathered.add(j + 1)
                        prep_gather(j + 1)
                    tGv = tG.t[:, 0:DC * nf].rearrange("p (dc f) -> p dc f", f=nf)
                    tUv = tU.t[:, 0:DC * nf].rearrange("p (dc f) -> p dc f", f=nf)
                    for fcl in range(nf // 128):
                        fc = f0 // 128 + fcl
                        pG = pb[(2 * fc) % 6]
                        pU = pb[(2 * fc + 1) % 6]
                        for (pp, tv, tt_) in ((pG, tGv, tG), (pU, tUv, tU)):
                            for dc in range(DC):
                                k.op("pe", lambda e: e.matmul(pp.t[:], lhsT=tv[:, dc, fcl * 128:(fcl + 1) * 128], rhs=XeT.t[:, dc, :],
                                                              start=(dc == 0), stop=(dc == DC - 1)), r=[tt_, XeT], w=[pp])
                        s_ = sg[fc % 2]
                        k.op("act", lambda e: e.activation(out=s_.t[:], in_=pG.t[:], func=AF.Silu), r=[pG], w=[s_])
                        k.op("dve", lambda e: e.tensor_tensor(out=HT.t[:, fc, :], in0=pU.t[:], in1=s_.t[:], op=ALU.mult),
                             r=[pU, s_], w=[HT])
                while deferred:
                    deferred.pop(0)()
                while pend:
                    tok_scatter(*pend.pop(0))
                if j + 1 < EL:
                    if (j + 1) not in gathered:
                        gathered.add(j + 1)
                        prep_gather(j + 1)
                    prep_transpose(j + 1)
                wdv = (wdb if pre else wd).t[j].rearrange("(fc p) n -> p fc n", p=128)
                for half in range(2):
                    c0 = half * 1024
                    for g0 in range(0, NFC, 8):
                        ng = min(8, NFC - g0)
                        tW = wq[cnt6["wn"] % 4]
                        k.dma(wq_eng, tW.t[:, 0:ng * 1024].rearrange("p (fc n) -> p fc n", n=1024), wdv[:, g0:g0 + ng, c0:c0 + 1024],
                              r=wsrc, w=[tW])
                        cnt6["wn"] += 1
                        tWv = tW.t[:, 0:ng * 1024].rearrange("p (fc n) -> p fc n", n=1024)
                        for fl in range(ng):
                            fc = g0 + fl
                            for st in range(4):
                                for nb in range(2):
                                    pp = pb[st * 2 + nb]
                                    k.op("pe", lambda e: e.matmul(pp.t[:], lhsT=HT.t[:, fc, st * 128:(st + 1) * 128],
                                                                  rhs=tWv[:, fl, nb * 512:(nb + 1) * 512], start=(fc == 0), stop=(fc == NFC - 1)),
                                         r=[HT, tW], w=[pp])
                    for st in range(4):
                        gate = tg[p][st].t[:, 1:2]
                        for nb in range(2):
                            pp = pb[st * 2 + nb]
                            osl = yst[st].t[:, c0 + nb * 512: c0 + (nb + 1) * 512]
                            if nb == 0:
                                k.op("act", lambda e: e.activation(out=osl, in_=pp.t[:], func=AF.Copy, scale=gate), r=[pp, tg[p][st]], w=[yst[st]])
                            else:
                                k.op("dve", lambda e: e.tensor_scalar(out=osl, in0=pp.t[:], scalar1=gate, scalar2=None, op0=ALU.mult),
                                     r=[pp, tg[p][st]], w=[yst[st]])
                def emit_scatter_add(p=p):
                    for st in range(4):
                        k.idma(out=yacc_sh.t[:, :], in_=yst[st].t[:, :], out_off=yri[p][st].t[:, 0:1], bounds=8191,
                               r=[yst[st], yri[p][st]], w=[yacc_sh], cop=ALU.add)
                if j + 1 < EL:
                    deferred.append(emit_scatter_add)
                else:
                    emit_scatter_add()
        k.pair_barrier(bsrc, bdst)

        with ExitStack() as es:
            g2b = k.tile(es, "g2b", [128, D], F32)
            fw = k.tile(es, "fw", [128, D], F32)
            load_bc(g2b, ada_d, 5 * D)
            load_bc(fw, fnw, 0)
            x1t = [k.tile(es, f"x7_{i}", [128, D], F32) for i in range(2)]
            ya = [k.tile(es, f"ya{i}", [128, D], F32) for i in range(2)]
            yb = [k.tile(es, f"yb{i}", [128, D], F32) for i in range(2)]
            ot = [k.tile(es, f"ot{i}", [128, D], F32) for i in range(2)]
            junk = k.tile(es, "junk7", [128, D], BF16)
            ss = k.tile(es, "ss7", [128, 2], F32)
            for tt in range(16):
                i = tt % 2
                k.dma("sp", x1t[i].t[:], x1_d.t[tt * 128:(tt + 1) * 128, :], r=[x1_d], w=[x1t[i]])
                k.idma(out=ya[i].t[:, :], in_=yacc_sh.t[:, :], in_off=idx_ya.t[:, tt:tt + 1], bounds=8191,
                       r=[yacc_sh, idx_ya], w=[ya[i]])
                k.idma(out=yb[i].t[:, :], in_=yacc_sh.t[:, :], in_off=idx_yb.t[:, tt:tt + 1], bounds=8191,
                       r=[yacc_sh, idx_yb], w=[yb[i]])
                k.op("dve", lambda e: e.tensor_tensor(out=ya[i].t[:], in0=ya[i].t[:], in1=yb[i].t[:], op=ALU.add),
                     r=[ya[i], yb[i]], w=[ya[i]])
                k.op("dve", lambda e: e.tensor_tensor(out=ya[i].t[:], in0=ya[i].t[:], in1=g2b.t[:], op=ALU.mult),
                     r=[ya[i], g2b], w=[ya[i]])
                k.op("dve", lambda e: e.tensor_tensor(out=ya[i].t[:], in0=ya[i].t[:], in1=x1t[i].t[:], op=ALU.add),
                     r=[ya[i], x1t[i]], w=[ya[i]])
                k.op("act", lambda e: e.activation(out=junk.t[:], in_=ya[i].t[:], func=AF.Square, accum_out=ss.t[:, 0:1]),
                     r=[ya[i]], w=[junk, ss])
                k.op("act", lambda e: e.activation(out=ss.t[:, 1:2], in_=ss.t[:, 0:1], func=AF.Sqrt, scale=1.0 / D, bias=epsb.t[:, 0:1]),
                     r=[ss, epsb], w=[ss])
                k.op("dve", lambda e: e.reciprocal(out=ss.t[:, 1:2], in_=ss.t[:, 1:2]), r=[ss], w=[ss])
                k.op("dve", lambda e: e.scalar_tensor_tensor(out=ot[i].t[:], in0=ya[i].t[:], scalar=ss.t[:, 1:2], in1=fw.t[:],
                                                             op0=ALU.mult, op1=ALU.mult), r=[ya[i], ss, fw], w=[ot[i]])
                k.dma("act", out.t[tt * 128:(tt + 1) * 128, :], ot[i].t[:], r=[ot[i]], w=[out])
        k.sync_all()
    return nc


def make_consts():
    i = np.arange(128)
    ident = np.eye(128, dtype=np.float32)
    triF = (i[:, None] <= i[None, :]).astype(np.float32)
    triB = (i[:, None] >= i[None, :]).astype(np.float32)
    ones = np.ones((128, 128), np.float32)
    strictL = (i[:, None] < i[None, :]).astype(np.float32)
    return np.ascontiguousarray(np.concatenate([ident, triF, triB, ones, strictL], axis=1))


def shard_inputs(x, c, ctx, c_ctx, w_ada, b_ada, norm1_w, w_in, mlstm_b_i, mlstm_b_f, mlstm_norm_w,
                 conv_w, conv_b, conv_norm_w, conv_norm_b, w_out, norm2_w, w_router, w_gate, w_up,
                 w_down, final_norm_w):
    f = np.float32
    consts = make_consts()
    p = np.arange(128)
    in_maps = []
    w_ada0 = np.ascontiguousarray(w_ada[0], f)
    w_out0 = np.ascontiguousarray(w_out[0], f)
    w_in0 = np.asarray(w_in[0], f)
    w_in_sw = w_in0.copy()
    w_in_sw[:, 4096:4104] = w_in0[:, 4104:4112]
    w_in_sw[:, 4104:4112] = w_in0[:, 4096:4104]
    wr_arr = np.ascontiguousarray(np.asarray(w_router[0], f).reshape(16, 128, 16).transpose(1, 0, 2))
    for core in range(8):
        b, r = core // 2, core % 2
        tau = np.arange(4096)
        posn = tau if r == 0 else 4095 - tau
        xall = np.ascontiguousarray(np.asarray(x[b], f)[posn])
        ctxl = np.asarray(ctx[b], f)
        ctxl = np.ascontiguousarray(ctxl if r == 0 else ctxl[::-1])
        cs2 = np.stack([np.asarray(c[b], f), np.asarray(c_ctx, f)], axis=-1)
        cs = np.ascontiguousarray(cs2.reshape(16, 128, 2).transpose(1, 0, 2))
        bi, bf_ = np.asarray(mlstm_b_i[0], f), np.asarray(mlstm_b_f[0], f)
        if r == 0:
            gbias = np.concatenate([bi[0], bf_[0], bi[1], bf_[1]])
        else:
            gbias = np.concatenate([bi[1], bf_[1], bi[0], bf_[0]])
        cwk = np.asarray(conv_w[0], f)
        if r == 1:
            cwk = cwk[::-1]
        convw = np.ascontiguousarray(cwk.T.reshape(8, 128, KT).transpose(1, 0, 2))

        def col8(v):
            return np.ascontiguousarray(np.asarray(v, f).reshape(8, 128).T)
        own_pos = posn[:NOWN]
        own_gid = np.ascontiguousarray(own_pos.reshape(16, 128).T.astype(np.int32))
        tp = (np.arange(32)[None, :] * 128 + p[:, None]).astype(np.int32)
        m = {
            "xall": xall, "ctxl": ctxl, "cs": cs,
            "w_ada": w_ada0, "b_ada": np.asarray(b_ada[0], f).reshape(1, -1),
            "n1w": np.asarray(norm1_w[0], f).reshape(1, -1), "n2w": np.asarray(norm2_w[0], f).reshape(1, -1),
            "fnw": np.asarray(final_norm_w, f).reshape(1, -1),
            "w_in": w_in0 if r == 0 else w_in_sw,
            "gbias": gbias.reshape(1, 16).astype(f),
            "mnw": np.asarray(mlstm_norm_w[0], f).reshape(1, -1),
            "convw": convw, "convb": col8(conv_b[0]), "cnw": col8(conv_norm_w[0]), "cnb": col8(conv_norm_b[0]),
            "w_out": w_out0, "w_r": wr_arr,
            "wg": np.asarray(w_gate[0, r * 8:(r + 1) * 8], f), "wu": np.asarray(w_up[0, r * 8:(r + 1) * 8], f),
            "wd": np.asarray(w_down[0, r * 8:(r + 1) * 8], f),
            "consts": consts,
            "own_gid": own_gid, "own_gid2": (own_gid + 4096).astype(np.int32),
            "zrows": (tp + r * 4096).astype(np.int32),
            "yrowsA": own_gid, "yrowsB": (own_gid + 4096).astype(np.int32),
            "affrows": (tp + r * 4096).astype(np.int32),
            "gid_f": tp.astype(np.float32),
            "ybase": np.full((128, 1), float(r * 4096), f),
        }
        in_maps.append(m)
    return in_maps


_NC_CACHE = {}


def kernel(**inputs):
    FF = int(np.asarray(inputs["w_gate"]).shape[-1])
    if FF not in _NC_CACHE:
        _NC_CACHE[FF] = build(FF)
    nc = _NC_CACHE[FF]
    in_maps = shard_inputs(**inputs)
    res = run_bass_kernel_spmd(nc, in_maps, core_ids=list(range(8)))
    B = np.asarray(inputs["x"]).shape[0]
    outp = np.zeros((B, 4096, D), np.float32)
    for core in range(8):
        b, r = core // 2, core % 2
        o = np.asarray(res.results[core]["out"], np.float32)
        if r == 0:
            outp[b, 0:NOWN] = o
        else:
            outp[b, 4095 - np.arange(NOWN)] = o
    return outp
```
